# Optimizing a Trainium2 kernel written in Bass

```python
import jax, jax.numpy as jnp
from jax import lax
import numpy as np

D_MODEL = 1024
BATCH = 16
SEQ = 2048
DEPTH = 1

ATT_HEADS = 8
ATT_KV_HEADS = 2
ATT_HEAD_DIM = 64
ATT_WIDTH = ATT_HEADS * ATT_HEAD_DIM
ATT_KV_WIDTH = ATT_KV_HEADS * ATT_HEAD_DIM
WINDOW = 128
ATT_BLOCK = 128
ROPE_THETA = 500000.0
ROT_DIM = ATT_HEAD_DIM // 4
ML_HEADS = 4
ML_QK_DIM = 64
ML_V_DIM = 128
ML_QK_WIDTH = ML_HEADS * ML_QK_DIM
ML_WIDTH = ML_HEADS * ML_V_DIM
ML_CHUNK = 64
GATE_CAP = 15.0
D_FF = ((8 * D_MODEL // 3 + 255) // 256) * 256
N_BRANCH = 2
EPS = 1e-6
IN_SIZES = (ATT_WIDTH, ATT_KV_WIDTH, ATT_KV_WIDTH,
            ML_QK_WIDTH, ML_QK_WIDTH, ML_WIDTH, ML_WIDTH,
            2 * ML_HEADS,
            N_BRANCH * D_MODEL)
IN_WIDTH = sum(IN_SIZES)

kernel_name = "hybrid_swa_sink_mlstm_gated_block"


def rmsnorm(x, g):
    xf = x.astype(jnp.float32)
    y = xf * lax.rsqrt(jnp.mean(xf * xf, axis=-1, keepdims=True) + EPS)
    return (y * g.astype(jnp.float32)).astype(x.dtype)


def rope_tables(positions):
    inv_freq = ROPE_THETA ** (-jnp.arange(0, ROT_DIM, 2, dtype=jnp.float32) / ROT_DIM)
    ang = positions.astype(jnp.float32)[..., None] * inv_freq
    return jnp.cos(ang)[:, :, None, :], jnp.sin(ang)[:, :, None, :]


def apply_partial_rope(x, cos, sin):
    half = ROT_DIM // 2
    xf = x.astype(jnp.float32)
    x1, x2, rest = xf[..., :half], xf[..., half:ROT_DIM], xf[..., ROT_DIM:]
    rot = jnp.concatenate([x1 * cos - x2 * sin, x2 * cos + x1 * sin, rest], axis=-1)
    return rot.astype(x.dtype)


def sliding_window_attention(q, k, v, sinks):
    B, S, Hq, hd = q.shape
    G = Hq // ATT_KV_HEADS
    nb = S // ATT_BLOCK
    qb = q.reshape(B, nb, ATT_BLOCK, ATT_KV_HEADS, G, hd)

    def band(a):
        ap = jnp.pad(a, ((0, 0), (ATT_BLOCK, 0), (0, 0), (0, 0)))
        ap = ap.reshape(B, nb + 1, ATT_BLOCK, ATT_KV_HEADS, hd)
        return jnp.concatenate([ap[:, :-1], ap[:, 1:]], axis=2)

    kb, vb = band(k), band(v)
    scores = jnp.einsum('bnqhgd,bnkhd->bnhgqk', qb, kb).astype(jnp.float32) * (hd ** -0.5)
    qi = jnp.arange(ATT_BLOCK)[:, None]
    kj = jnp.arange(2 * ATT_BLOCK)[None, :]
    rel = qi - kj + ATT_BLOCK
    in_window = (rel >= 0) & (rel < WINDOW)
    key_pos = jnp.arange(nb)[:, None, None] * ATT_BLOCK + kj[None] - ATT_BLOCK
    mask = in_window[None] & (key_pos >= 0)
    scores = jnp.where(mask[None, :, None, None], scores, -jnp.inf)
    sink = sinks.astype(jnp.float32).reshape(ATT_KV_HEADS, G)[None, None, :, :, None, None]
    m = jnp.maximum(scores.max(axis=-1, keepdims=True), sink)
    p = jnp.exp(scores - m)
    denom = p.sum(axis=-1, keepdims=True) + jnp.exp(sink - m)
    probs = (p / denom).astype(v.dtype)
    out = jnp.einsum('bnhgqk,bnkhd->bnqhgd', probs, vb)
    return out.reshape(B, S, Hq * hd)


def mlstm_chunkwise(q, k, v, i_pre, f_pre):
    B, S, H, dqk = q.shape
    dv = v.shape[-1]
    nc = S // ML_CHUNK
    f32 = jnp.float32
    q = q.astype(f32) * (dqk ** -0.5)
    k = k.astype(f32)
    v = v.astype(f32)
    i_log = GATE_CAP * jnp.tanh(i_pre.astype(f32) / GATE_CAP)
    f_log = jax.nn.log_sigmoid(GATE_CAP * jnp.tanh(f_pre.astype(f32) / GATE_CAP))

    def chunk4(a):
        return a.reshape(B, nc, ML_CHUNK, H, a.shape[-1]).transpose(1, 0, 3, 2, 4)

    def chunk3(a):
        return a.reshape(B, nc, ML_CHUNK, H).transpose(1, 0, 3, 2)

    causal = jnp.tril(jnp.ones((ML_CHUNK, ML_CHUNK), dtype=bool))

    def body(carry, xs):
        C, n, m = carry
        qc, kc, vc, ic, fc = xs
        b = jnp.cumsum(fc, axis=-1)
        Dm = jnp.where(causal, b[..., :, None] - b[..., None, :] + ic[..., None, :], -jnp.inf)
        inter = b + m[..., None]
        m_t = jnp.maximum(inter, Dm.max(axis=-1))
        w_intra = jnp.exp(Dm - m_t[..., None]) * jnp.einsum('bhtd,bhsd->bhts', qc, kc)
        w_inter = jnp.exp(inter - m_t)
        num = (jnp.einsum('bhts,bhsv->bhtv', w_intra, vc)
               + w_inter[..., None] * jnp.einsum('bhtd,bhdv->bhtv', qc, C))
        den = w_intra.sum(axis=-1) + w_inter * jnp.einsum('bhtd,bhd->bht', qc, n)
        h = num / jnp.maximum(jnp.abs(den), jnp.exp(-m_t))[..., None]
        b_last = b[..., -1]
        g = b_last[..., None] - b + ic
        m_new = jnp.maximum(b_last + m, g.max(axis=-1))
        decay = jnp.exp(b_last + m - m_new)
        wk = jnp.exp(g - m_new[..., None])
        C_new = decay[..., None, None] * C + jnp.einsum('bhs,bhsd,bhsv->bhdv', wk, kc, vc)
        n_new = decay[..., None] * n + jnp.einsum('bhs,bhsd->bhd', wk, kc)
        return (C_new, n_new, m_new), h

    init = (jnp.zeros((B, H, dqk, dv), f32), jnp.zeros((B, H, dqk), f32), jnp.zeros((B, H), f32))
    _, hs = lax.scan(body, init, (chunk4(q), chunk4(k), chunk4(v), chunk3(i_log), chunk3(f_log)))
    return hs.transpose(1, 0, 3, 2, 4).reshape(B, S, H, dv)


def setup_inputs(seed: int = 0) -> dict:
    key = jax.random.key(seed)
    ks = jax.random.split(key, 24)
    L = DEPTH

    def w(k, shape, fan_in):
        return jax.random.normal(k, shape, jnp.float32) * fan_in ** -0.5

    def gain(k, shape):
        return 1.0 + 0.05 * jax.random.normal(k, shape, jnp.float32)

    x = jax.random.normal(ks[0], (BATCH, SEQ, D_MODEL), jnp.float32)
    c = jax.random.normal(ks[1], (BATCH, D_MODEL), jnp.float32)
    offset = jax.random.randint(ks[2], (BATCH, 1), 0, 1024, dtype=jnp.int32)
    positions = offset + jnp.arange(SEQ, dtype=jnp.int32)[None, :]
    f_bias = jnp.linspace(3.0, 6.0, ML_HEADS, dtype=jnp.float32)[None, :] + 0.1 * jax.random.normal(ks[3], (L, ML_HEADS), jnp.float32)
    i_bias = 0.1 * jax.random.normal(ks[4], (L, ML_HEADS), jnp.float32)
    return {
        "x": x,
        "c": c,
        "positions": positions,
        "w_ada": w(ks[5], (L, D_MODEL, 6 * D_MODEL), D_MODEL),
        "b_ada": 0.02 * jax.random.normal(ks[6], (L, 6 * D_MODEL), jnp.float32),
        "g_pre_mix": gain(ks[7], (L, D_MODEL)),
        "w_in": w(ks[8], (L, D_MODEL, IN_WIDTH), D_MODEL),
        "b_mlstm_gates": jnp.concatenate([i_bias, f_bias], axis=-1),
        "sinks": 0.5 * jax.random.normal(ks[9], (L, ATT_HEADS), jnp.float32),
        "g_mlstm_norm": gain(ks[10], (L, ML_WIDTH)),
        "w_att_o": w(ks[11], (L, ATT_WIDTH, D_MODEL), ATT_WIDTH),
        "w_ml_o": w(ks[12], (L, ML_WIDTH, D_MODEL), ML_WIDTH),
        "w_out": w(ks[13], (L, D_MODEL, D_MODEL), D_MODEL),
        "g_post_mix": gain(ks[14], (L, D_MODEL)),
        "g_pre_ffn": gain(ks[15], (L, D_MODEL)),
        "w_ffn_in": w(ks[16], (L, D_MODEL, 2 * D_FF), D_MODEL),
        "w_ffn_out": w(ks[17], (L, D_FF, D_MODEL), D_FF),
        "g_post_ffn": gain(ks[18], (L, D_MODEL)),
    }


def reference(x, c, positions, w_ada, b_ada, g_pre_mix, w_in, b_mlstm_gates, sinks,
              g_mlstm_norm, w_att_o, w_ml_o, w_out, g_post_mix, g_pre_ffn, w_ffn_in,
              w_ffn_out, g_post_ffn):
    B, S, _ = x.shape
    cos, sin = rope_tables(positions)
    c_act = jax.nn.silu(c)
    split_idx = [int(v) for v in np.cumsum(IN_SIZES)[:-1]]
    for l in range(DEPTH):
        ada = c_act @ w_ada[l] + b_ada[l]
        shift1, scale1, gate1, shift2, scale2, gate2 = [a[:, None, :] for a in jnp.split(ada, 6, axis=-1)]

        h = rmsnorm(x, g_pre_mix[l]) * (1.0 + scale1) + shift1
        proj = h @ w_in[l]
        qa, ka, va, qm, km, vm, om, ifm, gates = jnp.split(proj, split_idx, axis=-1)

        qa = apply_partial_rope(qa.reshape(B, S, ATT_HEADS, ATT_HEAD_DIM), cos, sin)
        ka = apply_partial_rope(ka.reshape(B, S, ATT_KV_HEADS, ATT_HEAD_DIM), cos, sin)
        va = va.reshape(B, S, ATT_KV_HEADS, ATT_HEAD_DIM)
        att = sliding_window_attention(qa, ka, va, sinks[l])

        ifm = ifm + b_mlstm_gates[l]
        hm = mlstm_chunkwise(qm.reshape(B, S, ML_HEADS, ML_QK_DIM),
                             km.reshape(B, S, ML_HEADS, ML_QK_DIM),
                             vm.reshape(B, S, ML_HEADS, ML_V_DIM),
                             ifm[..., :ML_HEADS], ifm[..., ML_HEADS:])
        hm = hm * lax.rsqrt(jnp.mean(hm * hm, axis=-1, keepdims=True) + EPS)
        hm = hm.reshape(B, S, ML_WIDTH).astype(x.dtype) * g_mlstm_norm[l]
        ml = jax.nn.sigmoid(om) * hm

        g_att, g_ml = jnp.split(jax.nn.sigmoid(gates), N_BRANCH, axis=-1)
        merged = g_att * (att @ w_att_o[l]) + g_ml * (ml @ w_ml_o[l])
        mix = merged @ w_out[l]
        x = x + gate1 * rmsnorm(mix, g_post_mix[l])

        h2 = rmsnorm(x, g_pre_ffn[l]) * (1.0 + scale2) + shift2
        f_gate, f_up = jnp.split(h2 @ w_ffn_in[l], 2, axis=-1)
        ffn = (jax.nn.silu(f_gate) * f_up) @ w_ffn_out[l]
        x = x + gate2 * rmsnorm(ffn, g_post_ffn[l])
    return x
```

```python
import numpy as np
from contextlib import ExitStack
import concourse.bass as bass
import concourse.mybir as mybir
from concourse.bass_utils import run_bass_kernel_spmd

F32 = mybir.dt.float32
BF16 = mybir.dt.bfloat16
I32 = mybir.dt.int32
U8 = mybir.dt.uint8
AF = mybir.ActivationFunctionType
ALU = mybir.AluOpType

ENGS = ("pe", "act", "dve", "pool", "sp")

import os
MLG_EARLY = os.environ.get('K_MLG_EARLY', '1') == '1'
NCORES = 8
SEQ = 2048
D = 1024
NT = 32
TPS = 16
INW = 4360
DFF = 2816
NF = 22
EPS = 1e-6
PI = float(np.pi)


class Op:
    __slots__ = ("eng", "fn", "deps", "needs_inc", "count", "is_dma", "slot", "val")

    def __init__(self, eng, fn):
        self.eng = eng
        self.fn = fn
        self.deps = []
        self.needs_inc = False
        self.count = 0
        self.is_dma = False
        self.slot = None
        self.val = 0


class Prog:
    def __init__(self):
        self.ops = {e: [] for e in ENGS}
        self.last_w = {}
        self.reads = {}
        self.slot_cnt = {}
        self.pending = {e: [] for e in ENGS}
        self.all_dma = []
        self.final_waits = []
        self.stopped = False

    def _add(self, eng, fn, r, w, track_reads=True):
        o = Op(eng, fn)
        if self.stopped:
            return o
        deps = []
        pr = [k for k in r if k.startswith("ps") and k[2:].isdigit()]
        if pr:
            r = [k for k in r if k not in pr]
            w = list(w) + [k for k in pr if k not in w]
        for k in r:
            lw = self.last_w.get(k)
            if lw is not None:
                deps.append(lw)
        for k in w:
            lw = self.last_w.get(k)
            if lw is not None:
                deps.append(lw)
            deps.extend(self.reads.get(k, ()))
        deps.extend(self.pending[eng])
        self.pending[eng] = []
        seen = set()
        for d in deps:
            if id(d) in seen:
                continue
            seen.add(id(d))
            if eng == "pe" and d.eng == "pe" and not d.is_dma:
                continue
            o.deps.append(d)
        if track_reads:
            for k in r:
                self.reads.setdefault(k, []).append(o)
        for k in w:
            self.last_w[k] = o
            self.reads[k] = []
        self.ops[eng].append(o)
        return o

    def op(self, eng, fn, r=(), w=(), track_reads=True):
        return self._add(eng, fn, r, w, track_reads)

    def dma(self, queue, fn, slot, r=(), w=(), final=False):
        if self.stopped:
            return None
        o = self._add(queue, fn, r, w)
        o.is_dma = True
        o.slot = slot
        self.slot_cnt[slot] = self.slot_cnt.get(slot, 0) + 1
        o.val = 16 * self.slot_cnt[slot]
        self.all_dma.append(o)
        if final:
            self.final_waits.append(o)
        return o

    def barrier(self):
        lasts = [self.ops[e][-1] for e in ENGS if self.ops[e]]
        lasts = [o for o in lasts if not o.is_dma]
        dmas = list(self.all_dma)
        self.all_dma = []
        for e in ENGS:
            self.pending[e] = self.pending[e] + lasts + dmas

    def emit(self, nc, es):
        for e in ENGS:
            for o in self.ops[e]:
                for d in o.deps:
                    if not d.is_dma:
                        d.needs_inc = True
        esem = {e: es.enter_context(nc.semaphore("s_" + e)) for e in ENGS}
        ssem = {s: es.enter_context(nc.semaphore("d_%d" % i)) for i, s in enumerate(self.slot_cnt)}
        for e in ENGS:
            c = 0
            for o in self.ops[e]:
                if (not o.is_dma) and o.needs_inc:
                    c += 1
                    o.count = c
        ops = self.ops
        final_waits = self.final_waits

        def token(d):
            if d.is_dma:
                return ssem[d.slot], d.val
            return esem[d.eng], d.count

        def replay(e, h):
            waited = {}
            for o in ops[e]:
                for d in o.deps:
                    s, v = token(d)
                    if waited.get(id(s), 0) < v:
                        h.wait_ge(s, v)
                        waited[id(s)] = v
                ins = o.fn(h)
                if o.is_dma:
                    ins.then_inc(ssem[o.slot], 16)
                elif o.needs_inc:
                    ins.then_inc(esem[e], 1)
            if e == "sp":
                for d in final_waits:
                    s, v = token(d)
                    if waited.get(id(s), 0) < v:
                        h.wait_ge(s, v)
                        waited[id(s)] = v

        with nc.Block() as block:
            @block.tensor
            def _(h):
                replay("pe", h)

            @block.scalar
            def _(h):
                replay("act", h)

            @block.vector
            def _(h):
                replay("dve", h)

            @block.gpsimd
            def _(h):
                replay("pool", h)

            @block.sync
            def _(h):
                replay("sp", h)


DTSIZE = {F32: 4, BF16: 2, I32: 4, U8: 1}

C_ID, C_TRI, C_ONE, C_MW, C_MC, C_MP, C_INVF = 0, 128, 256, 384, 512, 640, 768
NCF = 776
V_BG, V_SINK, V_GN = 0, 8, 16
NVR = 528
V_GPM, V_GPF, V_BG1, V_BG2 = 0, 1024, 2048, 3072
NVS = 4096
VC_GPM, VC_GPF, VC_BSH1, VC_BSC1, VC_BSH2, VC_BSC2 = 0, 8, 16, 24, 32, 40
NVC = 48


def build_nc(n_tiles=NT, do_ffn=True, dbg=False, stop_at=None):
    nc = bass.Bass("TRN2", target_bir_lowering=False)
    ntok = NT * 128
    x_d = nc.dram_tensor("x", [ntok, D], F32, kind="ExternalInput").ap()
    pos_d = nc.dram_tensor("pos", [128, NT], I32, kind="ExternalInput").ap()
    ct_d = nc.dram_tensor("ct", [128, 8, 2], F32, kind="ExternalInput").ap()
    cf_d = nc.dram_tensor("cf", [128, NCF], F32, kind="ExternalInput").ap()
    vr_d = nc.dram_tensor("vr", [128, NVR], F32, kind="ExternalInput").ap()
    vs_d = nc.dram_tensor("vs", [128, NVS], F32, kind="ExternalInput").ap()
    vc_d = nc.dram_tensor("vc", [128, NVC], F32, kind="ExternalInput").ap()
    wada_d = nc.dram_tensor("w_ada", [D, 6 * D], F32, kind="ExternalInput").ap()
    win_d = nc.dram_tensor("w_in", [D, INW], F32, kind="ExternalInput").ap()
    wao_d = nc.dram_tensor("w_att_o", [512, D], F32, kind="ExternalInput").ap()
    wmo_d = nc.dram_tensor("w_ml_o", [512, D], F32, kind="ExternalInput").ap()
    wout_d = nc.dram_tensor("w_out", [D, D], F32, kind="ExternalInput").ap()
    wfi_d = nc.dram_tensor("w_ffn_in", [D, 2 * DFF], F32, kind="ExternalInput").ap()
    wfo_d = nc.dram_tensor("w_ffn_out", [DFF, D], F32, kind="ExternalInput").ap()
    out_d = nc.dram_tensor("out", [ntok, D], F32, kind="ExternalOutput").ap()
    x1_d = nc.dram_tensor("x1s", [ntok, D], F32, kind="Internal").ap()

    es = ExitStack()
    with es:
        WBYTES = (8 * 2 * DFF + NF * D) * 2
        ABYTES = 212863 - WBYTES - 64
        ABYTES = ABYTES // 32 * 32
        warena = es.enter_context(nc.sbuf_tensor("warena", [128, WBYTES], U8))
        arena = es.enter_context(nc.sbuf_tensor("arena", [128, ABYTES], U8))
        psum = es.enter_context(nc.psum_tensor("psum", [128, 4096], F32))
        off = [0]

        def carve(shape, dt, ar=None, offv=None):
            a_ = arena if ar is None else ar
            o_ = off if offv is None else offv
            n = int(np.prod(shape[1:])) * DTSIZE[dt]
            assert o_[0] + n <= (ABYTES if ar is None else WBYTES), (o_[0], n)
            a = a_[0:shape[0], o_[0]:o_[0] + n].bitcast(dt)
            o_[0] += (n + 31) // 32 * 32
            if len(shape) == 3:
                a = a.rearrange("p (a b) -> p a b", a=shape[1])
            elif len(shape) == 4:
                a = a.rearrange("p (a b c) -> p a b c", a=shape[1], b=shape[2])
            return a

        P = Prog()

        def ck(name):
            if stop_at is not None and name == stop_at:
                P.dma("sp", lambda h: h.dma_start(out=out_d[0:128, 0:16], in_=arena[:, 0:64].bitcast(F32)), "ckout", final=True)
                P.stopped = True

        bank_ctr = [0]

        def bank(n=1):
            b = bank_ctr[0]
            if n == 2 and b % 2 == 1:
                b += 1
            if b + n > 8:
                b = 0
            bank_ctr[0] = (b + n) % 8
            keys = ["ps%d" % (b + i) for i in range(n)]
            return psum[:, b * 512:(b + n) * 512], keys

        bank_free = [True] * 8
        bank_rr = [0]
        spin = [0]

        def gbank(n=1):
            while True:
                cand = None
                for d in range(8):
                    i = (bank_rr[0] + d) % 8
                    if i + n > 8:
                        continue
                    if all(bank_free[i + j] for j in range(n)):
                        cand = i
                        break
                if cand is not None:
                    break
                spin[0] += 1
                assert spin[0] < 100000, "PSUM allocator deadlock"
                yield
            spin[0] = 0
            for j in range(n):
                bank_free[cand + j] = False
            bank_rr[0] = (cand + n) % 8
            return psum[:, cand * 512:(cand + n) * 512], ["ps%d" % (cand + j) for j in range(n)]

        def rel(keys):
            for k in keys:
                bank_free[int(k[2:])] = True

        ident_b = carve([128, 128], BF16)
        A2c = carve([128, 8, 2], F32)
        S2c = carve([128, 8, 2], F32)
        G2r = carve([128, 2, D], F32)
        persistB_end = off[0]
        cf = carve([128, NCF], F32)
        vr = carve([128, NVR], F32)
        vc = carve([128, NVC], F32)
        ones_b = carve([128, 128], BF16)
        mcur_b = carve([128, 128], BF16)
        mprev_b = carve([128, 128], BF16)
        cosT = carve([128, NT, 8], F32)
        sinT = carve([128, NT, 8], F32)
        esink = carve([128, 8], F32)
        A1c = carve([128, 8, 2], F32)
        S1c = carve([128, 8, 2], F32)
        G1r = carve([128, 2, D], F32)
        persist_end = off[0]

        ident_f = cf[:, C_ID:C_ID + 128]
        tri_f = cf[:, C_TRI:C_TRI + 128]
        ones_f = cf[:, C_ONE:C_ONE + 128]
        maskw_f = cf[:, C_MW:C_MW + 128]

        P.dma("sp", lambda h: h.dma_start(out=cf, in_=cf_d), "cf", w=["cf"])
        P.dma("sp", lambda h: h.dma_start(out=vr, in_=vr_d), "vr", w=["vr"])
        P.dma("sp", lambda h: h.dma_start(out=vc, in_=vc_d), "vc", w=["vc"])
        P.op("dve", lambda h: h.tensor_copy(out=ident_b, in_=ident_f), r=["cf"], w=["ident_b"])
        P.op("dve", lambda h: h.tensor_copy(out=ones_b, in_=ones_f), r=["cf"], w=["ones_b"])
        P.op("dve", lambda h: h.tensor_copy(out=mcur_b, in_=cf[:, C_MC:C_MC + 128]), r=["cf"], w=["mcur_b"])
        P.op("dve", lambda h: h.tensor_copy(out=mprev_b, in_=cf[:, C_MP:C_MP + 128]), r=["cf"], w=["mprev_b"])
        P.op("act", lambda h: h.activation(out=esink, in_=vr[:, V_SINK:V_SINK + 8], func=AF.Exp), r=["vr"], w=["esink"])

        woff = [0]
        win_sb = carve([128, 8, INW], BF16, warena, woff)
        wao_sb = carve([128, 4, D], BF16, warena, woff)
        wmo_sb = carve([128, 4, D], BF16, warena, woff)
        wout_sb = carve([128, 8, D], BF16, warena, woff)

        posi = carve([128, NT], I32)
        posf = carve([128, NT], F32)
        ang = carve([128, NT, 8], F32)
        kk_i = carve([128, NT, 8], I32)
        kk_f = carve([128, NT, 8], F32)
        rr = carve([128, NT, 8], F32)
        rr2 = carve([128, NT, 8], F32)
        tmpa = carve([128, NT, 8], F32)
        ctf = carve([128, 8, 2], F32)
        ctb = carve([128, 8, 2], BF16)
        crep = carve([128, 8, 2, 128], BF16)
        wtail = woff[0]
        stage = [carve([128, 8, D], BF16, warena, woff), carve([128, 8, D], BF16)]
        vs = carve([128, NVS], F32)
        P.dma("sp", lambda h: h.dma_start(out=vs, in_=vs_d), "vs", w=["vs"])
        adat = carve([128, 8, 2], F32)
        gtmp = carve([128, D], F32)

        P.dma("sp", lambda h: h.dma_start(out=posi, in_=pos_d), "posi", w=["posi"])
        P.dma("sp", lambda h: h.dma_start(out=ctf, in_=ct_d), "ctf", w=["ctf"])
        ada_cols = [0, 1024, 2048, 3072, 4096, 5120]
        wada_v = wada_d.rearrange("(k p) n -> p k n", p=128)

        def load_stage(i):
            c0 = ada_cols[i]
            st = stage[i % 2]
            P.dma("pool", lambda h: h.dma_start(out=st, in_=wada_v[:, :, c0:c0 + D]), "stage%d" % (i % 2),
                  w=["stage%d" % (i % 2)])

        P.op("dve", lambda h: h.tensor_copy(out=posf, in_=posi), r=["posi"], w=["posf"])
        invf_b = cf[:, C_INVF:C_INVF + 8].unsqueeze(1).to_broadcast([128, NT, 8])
        P.op("dve", lambda h: h.tensor_tensor(out=ang, in0=posf.unsqueeze(2).to_broadcast([128, NT, 8]), in1=invf_b,
                                              op=ALU.mult), r=["posf", "cf"], w=["ang"])
        P.op("dve", lambda h: h.tensor_scalar(out=kk_f, in0=ang, scalar1=1.0 / (2 * PI), scalar2=None, op0=ALU.mult),
             r=["ang"], w=["kk_f"])
        P.op("dve", lambda h: h.tensor_copy(out=kk_i, in_=kk_f), r=["kk_f"], w=["kk_i"])
        P.op("dve", lambda h: h.tensor_copy(out=kk_f, in_=kk_i), r=["kk_i"], w=["kk_f"])
        P.op("dve", lambda h: h.scalar_tensor_tensor(out=rr, in0=kk_f, scalar=-2 * PI, in1=ang, op0=ALU.mult, op1=ALU.add),
             r=["kk_f", "ang"], w=["rr"])

        def wrap_sin(dst, shift, tag):
            P.op("dve", lambda h: h.tensor_scalar(out=rr2, in0=rr, scalar1=float(shift), scalar2=None, op0=ALU.add),
                 r=["rr"], w=["rr2"])
            P.op("dve", lambda h: h.tensor_scalar(out=tmpa, in0=rr2, scalar1=PI, scalar2=-2 * PI, op0=ALU.is_gt, op1=ALU.mult),
                 r=["rr2"], w=["tmpa"])
            P.op("dve", lambda h: h.tensor_tensor(out=rr2, in0=rr2, in1=tmpa, op=ALU.add), r=["rr2", "tmpa"], w=["rr2"])
            P.op("dve", lambda h: h.tensor_scalar(out=tmpa, in0=rr2, scalar1=-PI, scalar2=2 * PI, op0=ALU.is_lt, op1=ALU.mult),
                 r=["rr2"], w=["tmpa"])
            P.op("dve", lambda h: h.tensor_tensor(out=rr2, in0=rr2, in1=tmpa, op=ALU.add), r=["rr2", "tmpa"], w=["rr2"])
            P.op("act", lambda h: h.activation(out=dst, in_=rr2, func=AF.Sin), r=["rr2"], w=[tag])

        ck("consts")
        wrap_sin(sinT, 0.0, "sinT")
        wrap_sin(cosT, PI / 2, "cosT")

        ck("rope")
        P.op("act", lambda h: h.activation(out=ctb, in_=ctf, func=AF.Silu), r=["ctf"], w=["ctb"])
        for b in range(2):
            P.op("dve", lambda h, b=b: h.tensor_copy(out=crep[:, :, b, :], in_=ctb[:, :, b:b + 1].to_broadcast([128, 8, 128])),
                 r=["ctb"], w=["crep%d" % b])

        load_stage(0)
        load_stage(1)

        def ada_col(i, dst, bias_off, gain_off, plus_one):
            st = stage[i % 2]
            ps, pk = bank()
            for j in range(8):
                for k in range(8):
                    P.op("pe", lambda h, j=j, k=k: h.matmul(ps[:, j * 2:j * 2 + 2], lhsT=st[:, k, j * 128:(j + 1) * 128],
                                                            rhs=ctb[:, k, :], start=(k == 0), stop=(k == 7)),
                         r=["stage%d" % (i % 2), "ctb"], w=pk)
            psv = ps[:, 0:16].rearrange("p (j b) -> p j b", j=8)
            bia = vc[:, bias_off:bias_off + 8].unsqueeze(2).to_broadcast([128, 8, 2])
            P.op("dve", lambda h: h.tensor_tensor(out=adat, in0=psv, in1=bia, op=ALU.add), r=pk + ["vc"], w=["adat"])
            if plus_one:
                gn = vc[:, gain_off:gain_off + 8].unsqueeze(2).to_broadcast([128, 8, 2])
                P.op("dve", lambda h: h.scalar_tensor_tensor(out=dst, in0=adat, scalar=1.0, in1=gn, op0=ALU.add, op1=ALU.mult),
                     r=["adat", "vc"], w=["adacol%d" % i])
            else:
                P.op("dve", lambda h: h.tensor_copy(out=dst, in_=adat), r=["adat"], w=["adacol%d" % i])

        def ada_row(i, dst, bias_off, gain_off):
            st = stage[i % 2]
            for b in range(2):
                ps, pk = bank(2)
                for c in range(2):
                    for k in range(8):
                        P.op("pe", lambda h, b=b, c=c, k=k, ps=ps: h.matmul(ps[:, c * 512:(c + 1) * 512], lhsT=crep[:, k, b, :],
                                                                     rhs=st[:, k, c * 512:(c + 1) * 512],
                                                                     start=(k == 0), stop=(k == 7)),
                             r=["stage%d" % (i % 2), "crep%d" % b], w=[pk[c]])
                P.op("dve", lambda h, ps=ps: h.tensor_tensor(out=gtmp, in0=ps, in1=vs[:, bias_off:bias_off + D], op=ALU.add),
                     r=pk + ["vs"], w=["gtmp"])
                P.op("dve", lambda h, b=b: h.tensor_tensor(out=dst[:, b, :], in0=gtmp, in1=vs[:, gain_off:gain_off + D], op=ALU.mult),
                     r=["gtmp", "vs"], w=["adarow%d_%d" % (i, b)])

        ada_col(0, S1c, VC_BSH1, 0, False)
        load_stage(2)
        ada_col(1, A1c, VC_BSC1, VC_GPM, True)
        load_stage(3)
        win_v = win_d.rearrange("(k p) n -> p k n", p=128)
        for k in range(8):
            P.dma("pool", lambda h, k=k: h.dma_start(out=win_sb[:, k, :], in_=win_v[:, k, :]), "win%d" % k, w=["win%d" % k])
        ada_row(2, G1r, V_BG1, V_GPM)
        load_stage(4)
        ada_col(3, S2c, VC_BSH2, 0, False)
        load_stage(5)
        ada_col(4, A2c, VC_BSC2, VC_GPF, True)
        ada_row(5, G2r, V_BG2, V_GPF)
        wao_v = wao_d.rearrange("(k p) n -> p k n", p=128)
        wmo_v = wmo_d.rearrange("(k p) n -> p k n", p=128)
        wout_v = wout_d.rearrange("(k p) n -> p k n", p=128)
        P.dma("pool", lambda h: h.dma_start(out=wao_sb, in_=wao_v), "wao", w=["wao"])
        P.dma("pool", lambda h: h.dma_start(out=wmo_sb, in_=wmo_v), "wmo", w=["wmo"])
        P.dma("pool", lambda h: h.dma_start(out=wout_sb, in_=wout_v), "wout", w=["wout"])

        ck("setup")
        P.barrier()
        off[0] = persist_end
        WIN_KEYS = ["win%d" % k for k in range(8)]

        woff[0] = wtail
        X = [carve([128, D], F32, warena, woff), carve([128, D], F32, warena, woff)]
        x1t = [carve([128, D], F32, warena, woff), carve([128, D], F32, warena, woff)]
        sigG = carve([128, 2048], F32, warena, woff)
        t1 = carve([128, D], F32, warena, woff)
        junk = carve([128, D], BF16)
        st8 = carve([128, 16], F32)
        xs = carve([128, D], BF16)
        hT = carve([128, 8, 128], BF16)
        qk_tm = carve([128, 10, 64], BF16)
        rt = carve([128, 6, 10, 8], F32)
        qkf = carve([128, 10, 64], F32)
        qaT = carve([64, 8, 128], BF16)
        kaT = [carve([64, 2, 128], BF16), carve([64, 2, 128], BF16)]
        vaD = [carve([128, 2, 2, 64], BF16), carve([128, 2, 2, 64], BF16)]
        qm_tm = carve([128, 4, 64], BF16)
        km_tm = carve([128, 4, 64], BF16)
        qmT = carve([64, 4, 128], BF16)
        kmT = carve([64, 4, 128], BF16)
        qsT = carve([64, 4, 128], BF16)
        vm1 = carve([128, 4, 129], BF16)
        sigO = carve([128, 512], F32)
        gs = sigO.rearrange("p (h d) -> p h d", h=4)
        eT = [carve([128, 4, 128], BF16), carve([128, 4, 128], BF16)]
        pT = [carve([128, 4, 128], BF16), carve([128, 4, 128], BF16)]
        den2 = carve([128, 4, 128], F32)
        rec = den2
        attT = carve([128, 4, 128], BF16)
        gi = carve([128, 8], F32)
        th = carve([128, 8], F32)
        ef = carve([128, 4], F32)
        Fl = carve([128, 4], F32)
        imb = carve([128, 4], F32)
        Fm = carve([128, 4, 128], F32)
        Dexp = carve([128, 4, 128], F32)
        Eb = carve([64, 4, 128], F32)
        tmpW = Fm
        WT = carve([128, 4, 128], BF16)
        kw = carve([128, 4, 64], BF16)
        C1 = carve([64, 4, 129], F32)
        C1b = carve([64, 4, 129], BF16)
        dd = carve([128, 4], F32)
        ssq4 = carve([128, 4], F32)
        sm4 = carve([128, 4, 4], F32)
        ml = carve([128, 4, 128], BF16)
        mlT = carve([128, 4, 128], BF16)
        t2 = carve([128, D], F32)
        merged = carve([128, D], BF16)
        mT = carve([128, 8, 128], BF16)

        P.op("pool", lambda h: h.memset(vm1[:, :, 128:129], 1.0), w=["vm1"])

        def rsqrt_col(dst, src, scale, keys_r, key_w, tmpcol):
            P.op("act", lambda h: h.activation(out=tmpcol, in_=src, func=AF.Sqrt, scale=float(scale), bias=EPS),
                 r=keys_r, w=[key_w + "_sq"])
            P.op("dve", lambda h: h.reciprocal(out=dst, in_=tmpcol), r=[key_w + "_sq"], w=[key_w])

        x_v = x_d.rearrange("(t p) n -> t p n", p=128)
        x1_v = x1_d.rearrange("(t p) n -> t p n", p=128)
        out_v = out_d.rearrange("(t p) n -> t p n", p=128)

        def load_x(t):
            s = t % 2
            P.dma("sp", lambda h: h.dma_start(out=X[s], in_=x_v[t]), "X%d" % s, w=["X%d" % s])

        def make_tile(t):
            b = t // TPS
            n = t % TPS
            s = t % 2
            ks = n % 2
            Xk = "X%d" % s
            flags = {}

            def proj(c0, n_, ps_ap, keys):
                for k in range(8):
                    P.op("pe", lambda h, k=k: h.matmul(ps_ap[:, 0:n_], lhsT=hT[:, k, :], rhs=win_sb[:, k, c0:c0 + n_],
                                                       start=(k == 0), stop=(k == 7)),
                         r=["hT", "win%d" % k], w=keys, track_reads=True)

            def g_front():

                P.op("act", lambda h: h.activation(out=junk, in_=X[s], func=AF.Square, accum_out=st8[:, 0:1]),
                     r=[Xk], w=["junk", "ssq1"])
                yield
                rsqrt_col(st8[:, 2:3], st8[:, 0:1], 1.0 / D, ["ssq1"], "rstd1", st8[:, 1:2])
                yield
                P.op("dve", lambda h: h.tensor_scalar(out=xs, in0=X[s], scalar1=st8[:, 2:3], scalar2=None, op0=ALU.mult),
                     r=[Xk, "rstd1"], w=["xs"])
                yield
                ps, pk = yield from gbank()
                psb = ps.bitcast(BF16).rearrange("p (k n) -> p k n", k=8)
                for k in range(8):
                    P.op("pe", lambda h, k=k: h.transpose(out=psb[:, k, :], in_=xs[:, k * 128:(k + 1) * 128], identity=ident_b),
                         r=["xs", "ident_b"], w=pk)
                    yield
                for k in range(8):
                    P.op("act", lambda h, k=k: h.activation(out=hT[:, k, :], in_=psb[:, k, :], func=AF.Identity,
                                                            bias=S1c[:, k, b:b + 1], scale=A1c[:, k, b:b + 1]),
                         r=pk + ["adacol0", "adacol1"], w=["hT"])
                    yield
                rel(pk)
                ps_c, k_c = yield from gbank()
                proj(1024, 256, ps_c, k_c)
                yield
                proj(2304, 8, ps_c[:, 256:264], k_c)
                yield
                P.op("dve", lambda h: h.tensor_tensor(out=gi, in0=ps_c[:, 256:264], in1=vr[:, V_BG:V_BG + 8], op=ALU.add),
                     r=k_c + ["vr"], w=["gi"])
                yield
                ck("f1")
                flags["gi"] = True
                P.op("act", lambda h: h.copy(out=km_tm, in_=ps_c[:, 0:256].rearrange("p (h d) -> p h d", h=4)), r=k_c, w=["km_tm"])
                yield
                ck("f2")
                rel(k_c)
                ps_qa, k_qa = yield from gbank()
                proj(0, 512, ps_qa, k_qa)
                yield
                qa3 = ps_qa.rearrange("p (h d) -> p h d", h=8)
                P.op("act", lambda h: h.copy(out=qkf[:, 0:8, :], in_=qa3), r=k_qa, w=["qkf"])
                yield
                ck("f3")
                rel(k_qa)
                ps_b, k_b = yield from gbank()
                proj(512, 512, ps_b, k_b)
                yield
                ka3 = ps_b[:, 0:128].rearrange("p (h d) -> p h d", h=2)
                P.op("act", lambda h: h.copy(out=qkf[:, 8:10, :], in_=ka3), r=k_b, w=["qkf"])
                yield
                va3 = ps_b[:, 128:256].rearrange("p (h d) -> p h d", h=2)
                for dup in range(2):
                    P.op("dve", lambda h, dup=dup: h.tensor_copy(out=vaD[ks][:, :, dup, :], in_=va3), r=k_b, w=["vaD%d" % ks])
                    yield
                P.op("act", lambda h: h.copy(out=qm_tm, in_=ps_b[:, 256:512].rearrange("p (h d) -> p h d", h=4)), r=k_b, w=["qm_tm"])
                yield
                ck("f4")
                rel(k_b)
                P.op("pool", lambda h: h.tensor_copy(out=qk_tm, in_=qkf), r=["qkf"], w=["qk_tm"])
                yield
                ck("f5")
                ps_vm, k_vm = yield from gbank()
                proj(1280, 512, ps_vm, k_vm)
                yield
                P.op("dve", lambda h: h.tensor_copy(out=vm1[:, :, 0:128], in_=ps_vm.rearrange("p (h d) -> p h d", h=4)),
                     r=k_vm, w=["vm1"])
                yield
                ck("f6")
                rel(k_vm)
                ps_om, k_om = yield from gbank()
                proj(1792, 512, ps_om, k_om)
                yield
                P.op("act", lambda h: h.activation(out=sigO, in_=ps_om, func=AF.Sigmoid), r=k_om, w=["sigO"])
                yield
                rel(k_om)
                P.op("pool", lambda h: h.tensor_tensor(out=gs, in0=sigO.rearrange("p (h d) -> p h d", h=4),
                                                       in1=vr[:, V_GN:V_GN + 512].rearrange("p (h d) -> p h d", h=4), op=ALU.mult),
                     r=["sigO", "vr"], w=["sigO"])
                yield
                ck("f7")
                cs = cosT[:, t, :].unsqueeze(1)
                sn = sinT[:, t, :].unsqueeze(1)
                for (src, h0, nh, kk) in ((qkf[:, 0:8, :], 0, 8, ["qkf"]), (qkf[:, 8:10, :], 8, 2, ["qkf"])):
                    cb = cs.to_broadcast([128, nh, 8])
                    sb = sn.to_broadcast([128, nh, 8])
                    x1_ = src[:, :, 0:8]
                    x2_ = src[:, :, 8:16]
                    r0 = rt[:, 0, h0:h0 + nh, :]
                    r1 = rt[:, 1, h0:h0 + nh, :]
                    r2 = rt[:, 2, h0:h0 + nh, :]
                    r3 = rt[:, 3, h0:h0 + nh, :]
                    tg = "rt%d" % h0
                    P.op("dve", lambda h, r0=r0, x1_=x1_, cb=cb: h.tensor_tensor(out=r0, in0=x1_, in1=cb, op=ALU.mult),
                         r=kk + ["cosT"], w=[tg + "a"])
                    yield
                    P.op("dve", lambda h, r1=r1, x2_=x2_, sb=sb: h.tensor_tensor(out=r1, in0=x2_, in1=sb, op=ALU.mult),
                         r=kk + ["sinT"], w=[tg + "b"])
                    yield
                    P.op("dve", lambda h, r2=r2, x2_=x2_, cb=cb: h.tensor_tensor(out=r2, in0=x2_, in1=cb, op=ALU.mult),
                         r=kk + ["cosT"], w=[tg + "c"])
                    yield
                    P.op("dve", lambda h, r3=r3, x1_=x1_, sb=sb: h.tensor_tensor(out=r3, in0=x1_, in1=sb, op=ALU.mult),
                         r=kk + ["sinT"], w=[tg + "d"])
                    yield
                    P.op("dve", lambda h, r0=r0, r1=r1, h0=h0, nh=nh: h.tensor_tensor(out=qk_tm[:, h0:h0 + nh, 0:8], in0=r0, in1=r1,
                                                                                    op=ALU.subtract),
                         r=[tg + "a", tg + "b"], w=["qk_tm"])
                    yield
                    P.op("dve", lambda h, r2=r2, r3=r3, h0=h0, nh=nh: h.tensor_tensor(out=qk_tm[:, h0:h0 + nh, 8:16], in0=r2, in1=r3,
                                                                                    op=ALU.add),
                         r=[tg + "c", tg + "d"], w=["qk_tm"])
                    yield
                ps1, k1 = yield from gbank()
                p1b = ps1.bitcast(BF16)
                qaT_ps = p1b[0:64, :].rearrange("p (h n) -> p h n", h=8)
                for hh in range(8):
                    P.op("pe", lambda h, hh=hh: h.transpose(out=qaT_ps[:, hh, :], in_=qk_tm[:, hh, :], identity=ident_b),
                         r=["qk_tm", "ident_b"], w=k1)
                    yield
                P.op("act", lambda h: h.copy(out=qaT, in_=qaT_ps), r=k1, w=["qaT"])
                yield
                rel(k1)
                ps2, k2 = yield from gbank()
                p2b = ps2.bitcast(BF16)
                kaT_ps = p2b[0:64, 0:256].rearrange("p (h n) -> p h n", h=2)
                kmT_ps = p2b[0:64, 256:768].rearrange("p (h n) -> p h n", h=4)
                for hh in range(2):
                    P.op("pe", lambda h, hh=hh: h.transpose(out=kaT_ps[:, hh, :], in_=qk_tm[:, 8 + hh, :], identity=ident_b),
                         r=["qk_tm", "ident_b"], w=k2)
                    yield
                for hh in range(4):
                    P.op("pe", lambda h, hh=hh: h.transpose(out=kmT_ps[:, hh, :], in_=km_tm[:, hh, :], identity=ident_b),
                         r=["km_tm", "ident_b"], w=k2)
                    yield
                P.op("dve", lambda h: h.tensor_copy(out=kaT[ks], in_=kaT_ps), r=k2, w=["kaT%d" % ks])
                yield
                P.op("dve", lambda h: h.tensor_copy(out=kmT, in_=kmT_ps), r=k2, w=["kmT"])
                yield
                rel(k2)
                ps3, k3 = yield from gbank()
                p3b = ps3.bitcast(BF16)
                qmT_ps = p3b[0:64, 0:512].rearrange("p (h n) -> p h n", h=4)
                for hh in range(4):
                    P.op("pe", lambda h, hh=hh: h.transpose(out=qmT_ps[:, hh, :], in_=qm_tm[:, hh, :], identity=ident_b),
                         r=["qm_tm", "ident_b"], w=k3)
                    yield
                P.op("act", lambda h: h.copy(out=qmT, in_=qmT_ps), r=k3, w=["qmT"])
                yield
                rel(k3)


            def g_gates():

                for c in range(4):
                    psg, kg = yield from gbank()
                    proj(2312 + c * 512, 512, psg, kg)
                    yield
                    P.op("act", lambda h, c=c, psg=psg: h.activation(out=sigG[:, c * 512:(c + 1) * 512], in_=psg, func=AF.Sigmoid),
                         r=kg, w=["sigG%d" % c])
                    yield
                    rel(kg)


            def g_att(hk):

                blocks = [(ks, mcur_b, "mcur_b")]
                if n > 0:
                    blocks.append((1 - ks, mprev_b, "mprev_b"))
                ps_o, k_o = yield from gbank()
                ps_d, k_d = yield from gbank()
                for bi, (slot, mask, mkey) in enumerate(blocks):
                    ps_s, k_s = yield from gbank()
                    P.op("pe", lambda h, slot=slot, hk=hk, ps_s=ps_s: h.matmul(
                        ps_s, lhsT=kaT[slot][:, hk, :], rhs=qaT[:, 4 * hk:4 * hk + 4, :].rearrange("p a b -> p (a b)"), start=True, stop=True),
                        r=["kaT%d" % slot, "qaT"], w=k_s)
                    yield
                    P.op("act", lambda h, bi=bi, ps_s=ps_s: h.activation(out=eT[bi], in_=ps_s.rearrange("p (g n) -> p g n", g=4),
                                                                         func=AF.Exp, scale=0.125),
                         r=k_s, w=["eT%d" % bi])
                    yield
                    rel(k_s)
                    P.op("pool", lambda h, bi=bi, mask=mask: h.tensor_tensor(out=pT[bi], in0=eT[bi],
                                                                             in1=mask.unsqueeze(1).to_broadcast([128, 4, 128]),
                                                                             op=ALU.mult),
                         r=["eT%d" % bi, mkey], w=["pT%d" % bi])
                    yield
                    P.op("pe", lambda h, slot=slot, hk=hk, bi=bi, ps_o=ps_o: h.matmul(
                        ps_o, lhsT=vaD[slot][:, hk, :, :].rearrange("p a b -> p (a b)"), rhs=pT[bi].rearrange("p a b -> p (a b)"), start=(bi == 0), stop=(bi == len(blocks) - 1)),
                        r=["vaD%d" % slot, "pT%d" % bi], w=k_o)
                    yield
                    P.op("pe", lambda h, bi=bi, ps_d=ps_d: h.matmul(
                        ps_d, lhsT=ones_b, rhs=pT[bi].rearrange("p a b -> p (a b)"), start=(bi == 0), stop=(bi == len(blocks) - 1)),
                        r=["ones_b", "pT%d" % bi], w=k_d)
                    yield
                es4 = esink[:, 4 * hk:4 * hk + 4].unsqueeze(2).to_broadcast([128, 4, 128])
                P.op("dve", lambda h, ps_d=ps_d, es4=es4: h.tensor_tensor(out=den2, in0=ps_d.rearrange("p (g n) -> p g n", g=4),
                                                                         in1=es4, op=ALU.add),
                     r=k_d + ["esink"], w=["den2"])
                yield
                rel(k_d)
                P.op("act", lambda h: h.activation(out=rec, in_=den2, func=AF.Ln), r=["den2"], w=["den2"])
                yield
                P.op("act", lambda h: h.activation(out=rec, in_=rec, func=AF.Exp, scale=-1.0), r=["den2"], w=["den2"])
                yield
                o4 = ps_o.rearrange("p (j g n) -> p j g n", j=2, g=2)
                r4 = rec.rearrange("p (j g) n -> p j g n", j=2)
                for half in range(2):
                    lo, hi = half * 64, half * 64 + 64
                    for j in range(2):
                        P.op("dve", lambda h, lo=lo, hi=hi, half=half, j=j, o4=o4, r4=r4, hk=hk: h.tensor_tensor(
                            out=attT[lo:hi, 2 * hk + j, :], in0=o4[lo:hi, j, half, :], in1=r4[lo:hi, j, half, :], op=ALU.mult),
                            r=k_o + ["den2"], w=["attT"])
                        yield
                rel(k_o)


            def g_mlg():
                while not flags.get("gi"):
                    yield

                P.op("act", lambda h: h.activation(out=th, in_=gi, func=AF.Tanh, scale=1.0 / 15), r=["gi"], w=["th"])
                yield
                P.op("act", lambda h: h.activation(out=ef, in_=th[:, 4:8], func=AF.Exp, scale=-15.0), r=["th"], w=["ef"])
                yield
                P.op("act", lambda h: h.activation(out=ef, in_=ef, func=AF.Ln, bias=1.0), r=["ef"], w=["ef"])
                yield
                P.op("dve", lambda h: h.tensor_scalar(out=Fl, in0=ef, scalar1=-1.0, scalar2=None, op0=ALU.mult), r=["ef"], w=["Fl"])
                yield
                ps_cs, k_cs = yield from gbank()
                P.op("pe", lambda h: h.matmul(ps_cs[:, 0:4], lhsT=tri_f, rhs=Fl, start=True, stop=True), r=["cf", "Fl"], w=k_cs)
                yield
                P.op("dve", lambda h: h.scalar_tensor_tensor(out=imb, in0=th[:, 0:4], scalar=15.0, in1=ps_cs[:, 0:4],
                                                             op0=ALU.mult, op1=ALU.subtract), r=["th"] + k_cs, w=["imb"])
                yield
                rel(k_cs)
                P.op("dve", lambda h: h.tensor_tensor(out=Fm, in0=tri_f.unsqueeze(1).to_broadcast([128, 4, 128]),
                                                      in1=Fl.unsqueeze(2).to_broadcast([128, 4, 128]), op=ALU.mult),
                     r=["cf", "Fl"], w=["Fm"])
                yield
                ps_rb, k_rb = yield from gbank()
                P.op("pe", lambda h: h.matmul(ps_rb, lhsT=ones_f, rhs=Fm.rearrange("p a b -> p (a b)"), start=True, stop=True), r=["cf", "Fm"], w=k_rb)
                yield
                rb3 = ps_rb.rearrange("p (g n) -> p g n", g=4)
                for hh in range(4):
                    P.op("act", lambda h, hh=hh: h.activation(out=Dexp[:, hh, :], in_=rb3[:, hh, :], func=AF.Exp, bias=imb[:, hh:hh + 1]),
                         r=k_rb + ["imb"], w=["Dexp"])
                    yield
                P.op("act", lambda h: h.activation(out=Eb, in_=rb3[0:64], func=AF.Exp), r=k_rb, w=["Eb"])
                yield
                rel(k_rb)

            def g_ml():
                P.op("dve", lambda h: h.scalar_tensor_tensor(out=qsT, in0=qmT, scalar=0.125, in1=Eb, op0=ALU.mult, op1=ALU.mult),
                     r=["qmT", "Eb"], w=["qsT"])
                yield
                ps_st, k_st = yield from gbank()
                st3 = ps_st.rearrange("p (g n) -> p g n", g=4)
                for hh in range(4):
                    P.op("pe", lambda h, hh=hh: h.matmul(st3[:, hh, :], lhsT=kmT[:, hh, :], rhs=qmT[:, hh, :], start=True, stop=True),
                         r=["kmT", "qmT"], w=k_st)
                    yield
                P.op("pool", lambda h: h.tensor_tensor(out=tmpW, in0=Dexp, in1=maskw_f.unsqueeze(1).to_broadcast([128, 4, 128]),
                                                       op=ALU.mult), r=["Dexp", "cf"], w=["Fm"])
                yield
                P.op("dve", lambda h: h.tensor_tensor(out=WT, in0=tmpW, in1=st3, op=ALU.mult), r=["Fm"] + k_st, w=["WT"])
                yield
                rel(k_st)
                P.op("pool", lambda h: h.tensor_tensor(out=kw, in0=km_tm, in1=Dexp[:, :, 127:128].to_broadcast([128, 4, 64]),
                                                       op=ALU.mult), r=["km_tm", "Dexp"], w=["kw"])
                yield
                nums = []
                for pr in range(2):
                    ps_n, k_n = yield from gbank()
                    n3 = ps_n.rearrange("p (g n) -> p g n", g=2)[:, :, 0:129]
                    for g in range(2):
                        hh = 2 * pr + g
                        P.op("pe", lambda h, hh=hh, g=g, n3=n3: h.matmul(n3[:, g, :], lhsT=WT[:, hh, :], rhs=vm1[:, hh, :],
                                                                          start=True, stop=(n == 0)),
                             r=["WT", "vm1"], w=k_n)
                        yield
                        if n > 0:
                            P.op("pe", lambda h, hh=hh, g=g, n3=n3: h.matmul(n3[:, g, :], lhsT=qsT[:, hh, :], rhs=C1b[:, hh, :],
                                                                              start=False, stop=True),
                                 r=["qsT", "C1b"], w=k_n)
                            yield
                    nums.append((n3, k_n))
                cps = []
                for pr in range(2):
                    ps_c2, k_c2 = yield from gbank()
                    c3 = ps_c2[0:64, :].rearrange("p (g n) -> p g n", g=2)[:, :, 0:129]
                    for g in range(2):
                        hh = 2 * pr + g
                        P.op("pe", lambda h, hh=hh, g=g, c3=c3: h.matmul(c3[:, g, :], lhsT=kw[:, hh, :], rhs=vm1[:, hh, :],
                                                                          start=True, stop=True),
                             r=["kw", "vm1"], w=k_c2)
                        yield
                    cps.append((c3, k_c2))
                if n > 0:
                    P.op("dve", lambda h: h.tensor_tensor(out=C1, in0=C1, in1=Eb[:, :, 127:128].to_broadcast([64, 4, 129]), op=ALU.mult),
                         r=["C1", "Eb"], w=["C1"])
                    yield
                    for pr in range(2):
                        c3, k_c2 = cps[pr]
                        for g in range(2):
                            P.op("dve", lambda h, pr=pr, g=g, c3=c3: h.tensor_tensor(out=C1[:, 2 * pr + g, :], in0=C1[:, 2 * pr + g, :],
                                                                                     in1=c3[:, g, :], op=ALU.add), r=["C1"] + k_c2, w=["C1"])
                            yield
                else:
                    for pr in range(2):
                        c3, k_c2 = cps[pr]
                        for g in range(2):
                            P.op("dve", lambda h, pr=pr, g=g, c3=c3: h.tensor_copy(out=C1[:, 2 * pr + g, :], in_=c3[:, g, :]), r=k_c2, w=["C1"])
                            yield
                P.op("pool", lambda h: h.tensor_copy(out=C1b, in_=C1), r=["C1"], w=["C1b"])
                yield
                for (_c3, _k) in cps:
                    rel(_k)
                for pr in range(2):
                    n3, k_n = nums[pr]
                    for g in range(2):
                        P.op("dve", lambda h, pr=pr, g=g, n3=n3: h.tensor_copy(out=dd[:, 2 * pr + g:2 * pr + g + 1], in_=n3[:, g, 128:129]),
                             r=k_n, w=["dd"])
                        yield
                    for g in range(2):
                        hh = 2 * pr + g
                        P.op("act", lambda h, hh=hh, g=g, n3=n3: h.activation(out=junk[:, 0:128], in_=n3[:, g, 0:128], func=AF.Square,
                                                                              accum_out=ssq4[:, hh:hh + 1]),
                             r=k_n, w=["junk", "ssq4"])
                        yield
                sA = sm4[:, 0, :]
                sB = sm4[:, 1, :]
                sC = sm4[:, 2, :]
                sS = sm4[:, 3, :]
                P.op("dve", lambda h: h.scalar_tensor_tensor(out=dd, in0=dd, scalar=-1.0, in1=dd, op0=ALU.mult, op1=ALU.max), r=["dd"], w=["dd"])
                yield
                P.op("dve", lambda h: h.tensor_scalar(out=dd, in0=dd, scalar1=1.0, scalar2=None, op0=ALU.max), r=["dd"], w=["dd"])
                yield
                P.op("dve", lambda h: h.reciprocal(out=sA, in_=dd), r=["dd"], w=["sA"])
                yield
                P.op("dve", lambda h: h.tensor_tensor(out=sB, in0=sA, in1=sA, op=ALU.mult), r=["sA"], w=["sB"])
                yield
                P.op("dve", lambda h: h.tensor_tensor(out=sB, in0=sB, in1=ssq4, op=ALU.mult), r=["sB", "ssq4"], w=["sB"])
                yield
                P.op("act", lambda h: h.activation(out=sC, in_=sB, func=AF.Sqrt, scale=1.0 / 128, bias=EPS), r=["sB"], w=["sC"])
                yield
                P.op("dve", lambda h: h.reciprocal(out=sC, in_=sC), r=["sC"], w=["sC"])
                yield
                P.op("dve", lambda h: h.tensor_tensor(out=sS, in0=sA, in1=sC, op=ALU.mult), r=["sA", "sC"], w=["sS"])
                yield
                for pr in range(2):
                    n3, k_n = nums[pr]
                    for g in range(2):
                        hh = 2 * pr + g
                        P.op("dve", lambda h, hh=hh, g=g, n3=n3: h.scalar_tensor_tensor(out=ml[:, hh, :], in0=n3[:, g, 0:128],
                                                                                        scalar=sm4[:, 3, hh:hh + 1], in1=gs[:, hh, :],
                                                                                        op0=ALU.mult, op1=ALU.mult),
                             r=k_n + ["sS", "sigO"], w=["ml"])
                        yield
                for (_n3, _k) in nums:
                    rel(_k)
                ps_mt, k_mt = yield from gbank()
                mlT_ps = ps_mt.bitcast(BF16)[:, 0:512].rearrange("p (g n) -> p g n", g=4)
                for hh in range(4):
                    P.op("pe", lambda h, hh=hh: h.transpose(out=mlT_ps[:, hh, :], in_=ml[:, hh, :], identity=ident_b),
                         r=["ml", "ident_b"], w=k_mt)
                    yield
                P.op("act", lambda h: h.copy(out=mlT, in_=mlT_ps), r=k_mt, w=["mlT"])
                yield
                rel(k_mt)


            def g_tail():

                ps_a, k_a = yield from gbank(2)
                for c in range(2):
                    for j in range(4):
                        P.op("pe", lambda h, c=c, j=j: h.matmul(ps_a[:, c * 512:(c + 1) * 512], lhsT=attT[:, j, :],
                                                                rhs=wao_sb[:, j, c * 512:(c + 1) * 512], start=(j == 0), stop=(j == 3)),
                             r=["attT", "wao"], w=[k_a[c]])
                        yield
                ps_m, k_m = yield from gbank(2)
                for c in range(2):
                    for j in range(4):
                        P.op("pe", lambda h, c=c, j=j: h.matmul(ps_m[:, c * 512:(c + 1) * 512], lhsT=mlT[:, j, :],
                                                                rhs=wmo_sb[:, j, c * 512:(c + 1) * 512], start=(j == 0), stop=(j == 3)),
                             r=["mlT", "wmo"], w=[k_m[c]])
                        yield
                P.op("dve", lambda h: h.tensor_tensor(out=t1, in0=ps_a, in1=sigG[:, 0:1024], op=ALU.mult),
                     r=k_a + ["sigG0", "sigG1"], w=["t1"])
                yield
                rel(k_a)
                P.op("dve", lambda h: h.tensor_tensor(out=t2, in0=ps_m, in1=sigG[:, 1024:2048], op=ALU.mult),
                     r=k_m + ["sigG2", "sigG3"], w=["t2"])
                yield
                rel(k_m)
                P.op("pool", lambda h: h.tensor_tensor(out=merged, in0=t1, in1=t2, op=ALU.add), r=["t1", "t2"], w=["merged"])
                yield
                ps_t, k_t = yield from gbank()
                mT_ps = ps_t.bitcast(BF16).rearrange("p (k n) -> p k n", k=8)
                for k in range(8):
                    P.op("pe", lambda h, k=k: h.transpose(out=mT_ps[:, k, :], in_=merged[:, k * 128:(k + 1) * 128], identity=ident_b),
                         r=["merged", "ident_b"], w=k_t)
                    yield
                P.op("act", lambda h: h.copy(out=mT, in_=mT_ps), r=k_t, w=["mT"])
                yield
                rel(k_t)
                ps_x, k_x = yield from gbank(2)
                for c in range(2):
                    for k in range(8):
                        P.op("pe", lambda h, c=c, k=k: h.matmul(ps_x[:, c * 512:(c + 1) * 512], lhsT=mT[:, k, :],
                                                                rhs=wout_sb[:, k, c * 512:(c + 1) * 512], start=(k == 0), stop=(k == 7)),
                             r=["mT", "wout"], w=[k_x[c]])
                        yield
                P.op("act", lambda h: h.activation(out=junk, in_=ps_x, func=AF.Square, accum_out=st8[:, 4:5]), r=k_x, w=["junk", "ssq2"])
                yield
                rsqrt_col(st8[:, 6:7], st8[:, 4:5], 1.0 / D, ["ssq2"], "rstd2", st8[:, 5:6])
                yield
                P.op("dve", lambda h: h.scalar_tensor_tensor(out=t1, in0=ps_x, scalar=st8[:, 6:7], in1=G1r[:, b, :],
                                                             op0=ALU.mult, op1=ALU.mult),
                     r=k_x + ["rstd2", "adarow2_%d" % b], w=["t1"])
                yield
                rel(k_x)
                P.op("pool", lambda h: h.tensor_tensor(out=x1t[s], in0=X[s], in1=t1, op=ALU.add), r=[Xk, "t1"], w=["x1t%d" % s])
                yield
                P.dma("sp", lambda h: h.dma_start(out=x1_v[t], in_=x1t[s]), "x1t%d" % s, r=["x1t%d" % s], w=["x1d%d" % t],
                      final=(not do_ffn))
                yield


            return g_front, g_gates, g_att, g_ml, g_tail, g_mlg

        def interleave(gens):
            gens = list(gens)
            while gens:
                for g in list(gens):
                    try:
                        next(g)
                    except StopIteration:
                        gens.remove(g)

        load_x(0)
        prev_tail = None
        for t in range(n_tiles):
            g_front, g_gates, g_att, g_ml, g_tail, g_mlg = make_tile(t)
            first = [g_front()]
            if MLG_EARLY:
                first.append(g_mlg())
            if prev_tail is not None:
                first.insert(0, prev_tail())
            interleave(first)
            if t + 1 < n_tiles:
                load_x(t + 1)
            def g_att_all(g_att=g_att):
                yield from g_att(0)
                yield from g_att(1)
            def g_ml_all(g_mlg=g_mlg, g_ml=g_ml):
                if not MLG_EARLY:
                    yield from g_mlg()
                yield from g_ml()
            interleave([g_ml_all(), g_att_all(), g_gates()])
            prev_tail = g_tail
        interleave([prev_tail()])


        if do_ffn:
            P.barrier()
            for i_ in range(8):
                bank_free[i_] = True
            off[0] = persistB_end
            woff[0] = 0
            wfi_sb = carve([128, 8, 2 * DFF], BF16, warena, woff)
            wfo_sb = carve([128, NF, D], BF16, warena, woff)
            wfi_v = wfi_d.rearrange("(k p) n -> p k n", p=128)
            wfo_v = wfo_d.rearrange("(k p) n -> p k n", p=128)
            FB = 6
            blocks_f = [(f0, min(NF, f0 + FB)) for f0 in range(0, NF, FB)]
            for bi_, (f0, f1) in enumerate(blocks_f):
                for nm, cofs in (("wfg", 0), ("wfu", DFF)):
                    P.dma("pool", lambda h, f0=f0, f1=f1, cofs=cofs: h.dma_start(
                        out=wfi_sb[:, :, cofs + f0 * 128:cofs + f1 * 128], in_=wfi_v[:, :, cofs + f0 * 128:cofs + f1 * 128]),
                        "%s%d" % (nm, bi_), w=["%s%d" % (nm, bi_)])
            for bi_, (f0, f1) in enumerate(blocks_f):
                P.dma("pool", lambda h, f0=f0, f1=f1: h.dma_start(out=wfo_sb[:, f0:f1, :], in_=wfo_v[:, f0:f1, :]),
                      "wfo%d" % bi_, w=["wfo%d" % bi_])

            GT = 4
            XB = [carve([128, D], F32) for _ in range(2)]
            sb8 = carve([128, 16], F32)
            xs2 = carve([128, D], BF16)
            h2T = [carve([128, 8, GT * 128], BF16) for _ in range(2)]
            actT = carve([128, NF, GT * 128], BF16)
            silu = [carve([128, GT * 128], F32), carve([128, GT * 128], F32)]
            junk2 = silu[0].bitcast(BF16)
            ot = [carve([128, D], F32), carve([128, D], F32)]
            rx = carve([128, D], F32)

            def g_prep(g_):
                b = (g_ * GT) // TPS
                hb = g_ % 2
                for j in range(GT):
                    t = g_ * GT + j
                    xb = XB[j % 2]
                    xk = "XB%d" % (j % 2)
                    P.dma("sp", lambda h, t=t, xb=xb: h.dma_start(out=xb, in_=x1_v[t]), xk, r=["x1d%d" % t], w=[xk])
                    yield
                    P.op("act", lambda h, xb=xb: h.activation(out=xs2, in_=xb, func=AF.Square, accum_out=sb8[:, 0:1]),
                         r=[xk], w=["xs2", "b_ssq"])
                    yield
                    rsqrt_col(sb8[:, 2:3], sb8[:, 0:1], 1.0 / D, ["b_ssq"], "b_rstd", sb8[:, 1:2])
                    yield
                    P.op("dve", lambda h, xb=xb: h.tensor_scalar(out=xs2, in0=xb, scalar1=sb8[:, 2:3], scalar2=None, op0=ALU.mult),
                         r=[xk, "b_rstd"], w=["xs2"])
                    yield
                    ps, pk = yield from gbank()
                    psb = ps.bitcast(BF16).rearrange("p (k n) -> p k n", k=8)
                    for k in range(8):
                        P.op("pe", lambda h, k=k, psb=psb: h.transpose(out=psb[:, k, :], in_=xs2[:, k * 128:(k + 1) * 128], identity=ident_b),
                             r=["xs2", "ident_b"], w=pk)
                        yield
                    for k in range(8):
                        P.op("act", lambda h, k=k, j=j, psb=psb, b=b, hb=hb: h.activation(
                            out=h2T[hb][:, k, j * 128:(j + 1) * 128], in_=psb[:, k, :], func=AF.Identity,
                            bias=S2c[:, k, b:b + 1], scale=A2c[:, k, b:b + 1]),
                            r=pk + ["adacol3", "adacol4"], w=["h2T%d" % hb])
                        yield
                    rel(pk)

            def g_ffn(g_):
                b = (g_ * GT) // TPS
                hb = g_ % 2
                hk_ = "h2T%d" % hb
                for f in range(NF):
                    blk = f // FB
                    ps_g, k_g = yield from gbank()
                    for k in range(8):
                        P.op("pe", lambda h, k=k, f=f, ps_g=ps_g: h.matmul(ps_g, lhsT=wfi_sb[:, k, f * 128:(f + 1) * 128],
                                                                           rhs=h2T[hb][:, k, :], start=(k == 0), stop=(k == 7)),
                             r=["wfg%d" % blk, hk_], w=k_g)
                    yield
                    ps_u, k_u = yield from gbank()
                    for k in range(8):
                        P.op("pe", lambda h, k=k, f=f, ps_u=ps_u: h.matmul(ps_u, lhsT=wfi_sb[:, k, DFF + f * 128:DFF + (f + 1) * 128],
                                                                           rhs=h2T[hb][:, k, :], start=(k == 0), stop=(k == 7)),
                             r=["wfu%d" % blk, hk_], w=k_u)
                    yield
                    sl = silu[f % 2]
                    P.op("act", lambda h, sl=sl, ps_g=ps_g: h.activation(out=sl, in_=ps_g, func=AF.Silu), r=k_g, w=["silu%d" % (f % 2)])
                    yield
                    rel(k_g)
                    P.op("dve", lambda h, sl=sl, f=f, ps_u=ps_u: h.tensor_tensor(out=actT[:, f, :], in0=sl, in1=ps_u, op=ALU.mult),
                         r=["silu%d" % (f % 2)] + k_u, w=["actT%d" % f])
                    yield
                    rel(k_u)
                for j in range(GT):
                    t = g_ * GT + j
                    ps_y, k_y = yield from gbank(2)
                    for c in range(2):
                        for f in range(NF):
                            P.op("pe", lambda h, c=c, f=f, j=j, ps_y=ps_y: h.matmul(
                                ps_y[:, c * 512:(c + 1) * 512], lhsT=actT[:, f, j * 128:(j + 1) * 128],
                                rhs=wfo_sb[:, f, c * 512:(c + 1) * 512], start=(f == 0), stop=(f == NF - 1)),
                                r=["actT%d" % f, "wfo%d" % (f // FB)], w=[k_y[c]])
                            if f % 4 == 3:
                                yield
                    P.dma("sp", lambda h, t=t: h.dma_start(out=rx, in_=x1_v[t]), "rx", r=["x1d%d" % t], w=["rx"])
                    yield
                    P.op("act", lambda h, ps_y=ps_y: h.activation(out=junk2, in_=ps_y, func=AF.Square, accum_out=sb8[:, 4:5]),
                         r=k_y, w=["silu0", "b_ssq2"])
                    yield
                    rsqrt_col(sb8[:, 6:7], sb8[:, 4:5], 1.0 / D, ["b_ssq2"], "b_rstd2", sb8[:, 5:6])
                    yield
                    o_ = ot[j % 2]
                    P.op("dve", lambda h, ps_y=ps_y, o_=o_, b=b: h.scalar_tensor_tensor(out=o_, in0=ps_y, scalar=sb8[:, 6:7], in1=G2r[:, b, :],
                                                                                   op0=ALU.mult, op1=ALU.mult),
                         r=k_y + ["b_rstd2", "adarow5_%d" % b], w=["ot%d" % (j % 2)])
                    yield
                    rel(k_y)
                    P.op("pool", lambda h, o_=o_: h.tensor_tensor(out=o_, in0=rx, in1=o_, op=ALU.add),
                         r=["rx", "ot%d" % (j % 2)], w=["ot%d" % (j % 2)])
                    yield
                    P.dma("sp", lambda h, o_=o_, t=t: h.dma_start(out=out_v[t], in_=o_), "ot%d" % (j % 2), r=["ot%d" % (j % 2)],
                          final=True)
                    yield

            n_groups = n_tiles // GT
            interleave([g_prep(0)])
            for g_ in range(n_groups):
                th_ = [g_ffn(g_)]
                if g_ + 1 < n_groups:
                    th_.append(g_prep(g_ + 1))
                interleave(th_)

        P.emit(nc, es)
    return nc


def _host_layout(inputs):
    f32 = np.float32
    x = np.asarray(inputs["x"], dtype=f32)
    c = np.asarray(inputs["c"], dtype=f32)
    pos = np.asarray(inputs["positions"], dtype=np.int32)
    cf = np.zeros((128, NCF), f32)
    ii = np.arange(128)
    cf[:, C_ID:C_ID + 128] = np.eye(128, dtype=f32)
    cf[:, C_TRI:C_TRI + 128] = (ii[:, None] <= ii[None, :]).astype(f32)
    cf[:, C_ONE:C_ONE + 128] = 1.0
    cf[:, C_MW:C_MW + 128] = 0.125 * (ii[:, None] <= ii[None, :]).astype(f32)
    cf[:, C_MC:C_MC + 128] = (ii[:, None] <= ii[None, :]).astype(f32)
    cf[:, C_MP:C_MP + 128] = (ii[:, None] > ii[None, :]).astype(f32)
    inv_freq = (np.float32(500000.0) ** (-np.arange(0, 16, 2, dtype=f32) / np.float32(16))).astype(f32)
    cf[:, C_INVF:C_INVF + 8] = inv_freq[None, :]

    def rep(v):
        return np.broadcast_to(np.asarray(v, f32).reshape(1, -1), (128, np.asarray(v).size))

    def col(v):
        return np.asarray(v, f32).reshape(-1, 128).T

    b_ada = np.asarray(inputs["b_ada"], f32)[0]
    vr = np.zeros((128, NVR), f32)
    vr[:, V_BG:V_BG + 8] = rep(inputs["b_mlstm_gates"][0])
    vr[:, V_SINK:V_SINK + 8] = rep(inputs["sinks"][0])
    vr[:, V_GN:V_GN + 512] = rep(inputs["g_mlstm_norm"][0])
    vs = np.zeros((128, NVS), f32)
    vs[:, V_GPM:V_GPM + D] = rep(inputs["g_post_mix"][0])
    vs[:, V_GPF:V_GPF + D] = rep(inputs["g_post_ffn"][0])
    vs[:, V_BG1:V_BG1 + D] = rep(b_ada[2048:3072])
    vs[:, V_BG2:V_BG2 + D] = rep(b_ada[5120:6144])
    vc = np.zeros((128, NVC), f32)
    vc[:, VC_GPM:VC_GPM + 8] = col(inputs["g_pre_mix"][0])
    vc[:, VC_GPF:VC_GPF + 8] = col(inputs["g_pre_ffn"][0])
    vc[:, VC_BSH1:VC_BSH1 + 8] = col(b_ada[0:1024])
    vc[:, VC_BSC1:VC_BSC1 + 8] = col(b_ada[1024:2048])
    vc[:, VC_BSH2:VC_BSH2 + 8] = col(b_ada[3072:4096])
    vc[:, VC_BSC2:VC_BSC2 + 8] = col(b_ada[4096:5120])
    shared = dict(
        cf=cf, vr=vr, vc=vc, vs=vs,
        w_ada=np.ascontiguousarray(inputs["w_ada"][0], dtype=f32),
        w_in=np.ascontiguousarray(inputs["w_in"][0], dtype=f32),
        w_att_o=np.ascontiguousarray(inputs["w_att_o"][0], dtype=f32),
        w_ml_o=np.ascontiguousarray(inputs["w_ml_o"][0], dtype=f32),
        w_out=np.ascontiguousarray(inputs["w_out"][0], dtype=f32),
        w_ffn_in=np.ascontiguousarray(inputs["w_ffn_in"][0], dtype=f32),
        w_ffn_out=np.ascontiguousarray(inputs["w_ffn_out"][0], dtype=f32),
    )
    in_maps = []
    for i in range(NCORES):
        xb = x[2 * i:2 * i + 2].reshape(2 * SEQ, D)
        pb = pos[2 * i:2 * i + 2].reshape(NT, 128).T
        cb = c[2 * i:2 * i + 2]
        ct = cb.reshape(2, 8, 128).transpose(2, 1, 0)
        m = dict(shared)
        m["x"] = np.ascontiguousarray(xb)
        m["pos"] = np.ascontiguousarray(pb, dtype=np.int32)
        m["ct"] = np.ascontiguousarray(ct, dtype=f32)
        in_maps.append(m)
    return in_maps


_NC_CACHE = {}


def kernel(**inputs):
    in_maps = _host_layout(inputs)
    if "nc" not in _NC_CACHE:
        _NC_CACHE["nc"] = build_nc()
    nc = _NC_CACHE["nc"]
    res = run_bass_kernel_spmd(nc, in_maps, core_ids=list(range(NCORES)))
    outs = [np.asarray(r["out"], dtype=np.float32).reshape(2, SEQ, D) for r in res.results]
    return np.concatenate(outs, axis=0)
```

```python
import numpy as np
from contextlib import ExitStack
import concourse.bass as bass
import concourse.mybir as mybir
from concourse.bass_utils import run_bass_kernel_spmd

F32 = mybir.dt.float32
BF16 = mybir.dt.bfloat16
I32 = mybir.dt.int32
U8 = mybir.dt.uint8
AF = mybir.ActivationFunctionType
ALU = mybir.AluOpType

ENGS = ("pe", "act", "dve", "pool", "sp")

import os
MLG_EARLY = os.environ.get('K_MLG_EARLY', '1') == '1'
NCORES = 8
SEQ = 2048
D = 1024
NT = 32
TPS = 16
INW = 4360
DFF = 2816
NF = 22
EPS = 1e-6
PI = float(np.pi)


class Op:
    __slots__ = ("eng", "fn", "deps", "needs_inc", "count", "is_dma", "slot", "val")

    def __init__(self, eng, fn):
        self.eng = eng
        self.fn = fn
        self.deps = []
        self.needs_inc = False
        self.count = 0
        self.is_dma = False
        self.slot = None
        self.val = 0


class Prog:
    def __init__(self):
        self.ops = {e: [] for e in ENGS}
        self.last_w = {}
        self.reads = {}
        self.slot_cnt = {}
        self.pending = {e: [] for e in ENGS}
        self.all_dma = []
        self.final_waits = []
        self.stopped = False

    def _add(self, eng, fn, r, w, track_reads=True):
        o = Op(eng, fn)
        if self.stopped:
            return o
        deps = []
        pr = [k for k in r if k.startswith("ps") and k[2:].isdigit()]
        if pr:
            r = [k for k in r if k not in pr]
            w = list(w) + [k for k in pr if k not in w]
        for k in r:
            lw = self.last_w.get(k)
            if lw is not None:
                deps.append(lw)
        for k in w:
            lw = self.last_w.get(k)
            if lw is not None:
                deps.append(lw)
            deps.extend(self.reads.get(k, ()))
        deps.extend(self.pending[eng])
        self.pending[eng] = []
        seen = set()
        for d in deps:
            if id(d) in seen:
                continue
            seen.add(id(d))
            if eng == "pe" and d.eng == "pe" and not d.is_dma:
                continue
            o.deps.append(d)
        if track_reads:
            for k in r:
                self.reads.setdefault(k, []).append(o)
        for k in w:
            self.last_w[k] = o
            self.reads[k] = []
        self.ops[eng].append(o)
        return o

    def op(self, eng, fn, r=(), w=(), track_reads=True):
        return self._add(eng, fn, r, w, track_reads)

    def dma(self, queue, fn, slot, r=(), w=(), final=False):
        if self.stopped:
            return None
        o = self._add(queue, fn, r, w)
        o.is_dma = True
        o.slot = slot
        self.slot_cnt[slot] = self.slot_cnt.get(slot, 0) + 1
        o.val = 16 * self.slot_cnt[slot]
        self.all_dma.append(o)
        if final:
            self.final_waits.append(o)
        return o

    def barrier(self):
        lasts = [self.ops[e][-1] for e in ENGS if self.ops[e]]
        lasts = [o for o in lasts if not o.is_dma]
        dmas = list(self.all_dma)
        self.all_dma = []
        for e in ENGS:
            self.pending[e] = self.pending[e] + lasts + dmas

    def emit(self, nc, es):
        for e in ENGS:
            for o in self.ops[e]:
                for d in o.deps:
                    if not d.is_dma:
                        d.needs_inc = True
        esem = {e: es.enter_context(nc.semaphore("s_" + e)) for e in ENGS}
        ssem = {s: es.enter_context(nc.semaphore("d_%d" % i)) for i, s in enumerate(self.slot_cnt)}
        for e in ENGS:
            c = 0
            for o in self.ops[e]:
                if (not o.is_dma) and o.needs_inc:
                    c += 1
                    o.count = c
        ops = self.ops
        final_waits = self.final_waits

        def token(d):
            if d.is_dma:
                return ssem[d.slot], d.val
            return esem[d.eng], d.count

        def replay(e, h):
            waited = {}
            for o in ops[e]:
                for d in o.deps:
                    s, v = token(d)
                    if waited.get(id(s), 0) < v:
                        h.wait_ge(s, v)
                        waited[id(s)] = v
                ins = o.fn(h)
                if o.is_dma:
                    ins.then_inc(ssem[o.slot], 16)
                elif o.needs_inc:
                    ins.then_inc(esem[e], 1)
            if e == "sp":
                for d in final_waits:
                    s, v = token(d)
                    if waited.get(id(s), 0) < v:
                        h.wait_ge(s, v)
                        waited[id(s)] = v

        with nc.Block() as block:
            @block.tensor
            def _(h):
                replay("pe", h)

            @block.scalar
            def _(h):
                replay("act", h)

            @block.vector
            def _(h):
                replay("dve", h)

            @block.gpsimd
            def _(h):
                replay("pool", h)

            @block.sync
            def _(h):
                replay("sp", h)


DTSIZE = {F32: 4, BF16: 2, I32: 4, U8: 1}

C_TRI, C_ONE, C_MW, C_ID, C_MC, C_MP, C_INVF = 0, 128, 256, 384, 512, 640, 768
NCP = 384
NCF = 776
V_BG, V_SINK, V_GN = 0, 8, 16
NVR = 528
V_GPM, V_GPF, V_BG1, V_BG2 = 0, 1024, 2048, 3072
NVS = 4096
VC_GPM, VC_GPF, VC_BSH1, VC_BSC1, VC_BSH2, VC_BSC2 = 0, 8, 16, 24, 32, 40
NVC = 48


def build_nc(n_tiles=NT, do_ffn=True, dbg=False, stop_at=None):
    nc = bass.Bass("TRN2", target_bir_lowering=False)
    ntok = NT * 128
    x_d = nc.dram_tensor("x", [ntok, D], F32, kind="ExternalInput").ap()
    pos_d = nc.dram_tensor("pos", [128, NT], I32, kind="ExternalInput").ap()
    ct_d = nc.dram_tensor("ct", [128, 8, 2], F32, kind="ExternalInput").ap()
    cf_d = nc.dram_tensor("cf", [128, NCF], F32, kind="ExternalInput").ap()
    vr_d = nc.dram_tensor("vr", [128, NVR], F32, kind="ExternalInput").ap()
    vs_d = nc.dram_tensor("vs", [128, NVS], F32, kind="ExternalInput").ap()
    vc_d = nc.dram_tensor("vc", [128, NVC], F32, kind="ExternalInput").ap()
    wada_d = nc.dram_tensor("w_ada", [D, 6 * D], F32, kind="ExternalInput").ap()
    win_d = nc.dram_tensor("w_in", [D, INW], F32, kind="ExternalInput").ap()
    wao_d = nc.dram_tensor("w_att_o", [512, D], F32, kind="ExternalInput").ap()
    wmo_d = nc.dram_tensor("w_ml_o", [512, D], F32, kind="ExternalInput").ap()
    wout_d = nc.dram_tensor("w_out", [D, D], F32, kind="ExternalInput").ap()
    wfi_d = nc.dram_tensor("w_ffn_in", [D, 2 * DFF], F32, kind="ExternalInput").ap()
    wfo_d = nc.dram_tensor("w_ffn_out", [DFF, D], F32, kind="ExternalInput").ap()
    out_d = nc.dram_tensor("out", [ntok, D], F32, kind="ExternalOutput").ap()
    x1_d = nc.dram_tensor("x1s", [ntok, D], F32, kind="Internal").ap()

    es = ExitStack()
    with es:
        WBYTES = (8 * 2 * DFF + NF * D) * 2
        ABYTES = 212863 - WBYTES - 64
        ABYTES = ABYTES // 32 * 32
        warena = es.enter_context(nc.sbuf_tensor("warena", [128, WBYTES], U8))
        arena = es.enter_context(nc.sbuf_tensor("arena", [128, ABYTES], U8))
        psum = es.enter_context(nc.psum_tensor("psum", [128, 4096], F32))
        off = [0]

        def carve(shape, dt, ar=None, offv=None):
            a_ = arena if ar is None else ar
            o_ = off if offv is None else offv
            n = int(np.prod(shape[1:])) * DTSIZE[dt]
            assert o_[0] + n <= (ABYTES if ar is None else WBYTES), (o_[0], n)
            a = a_[0:shape[0], o_[0]:o_[0] + n].bitcast(dt)
            o_[0] += (n + 31) // 32 * 32
            if len(shape) == 3:
                a = a.rearrange("p (a b) -> p a b", a=shape[1])
            elif len(shape) == 4:
                a = a.rearrange("p (a b c) -> p a b c", a=shape[1], b=shape[2])
            return a

        P = Prog()

        def ck(name):
            if stop_at is not None and name == stop_at:
                P.dma("sp", lambda h: h.dma_start(out=out_d[0:128, 0:16], in_=arena[:, 0:64].bitcast(F32)), "ckout", final=True)
                P.stopped = True

        bank_ctr = [0]

        def bank(n=1):
            b = bank_ctr[0]
            if n == 2 and b % 2 == 1:
                b += 1
            if b + n > 8:
                b = 0
            bank_ctr[0] = (b + n) % 8
            keys = ["ps%d" % (b + i) for i in range(n)]
            return psum[:, b * 512:(b + n) * 512], keys

        bank_free = [True] * 8
        bank_rr = [0]
        spin = [0]

        def gbank(n=1):
            while True:
                cand = None
                if n == 2:
                    if bank_free[6] and bank_free[7]:
                        cand = 6
                else:
                    for d in range(6):
                        i = (bank_rr[0] + d) % 6
                        if bank_free[i]:
                            cand = i
                            break
                if cand is not None:
                    break
                spin[0] += 1
                assert spin[0] < 200000, "PSUM allocator deadlock"
                yield
            spin[0] = 0
            for j in range(n):
                bank_free[cand + j] = False
            if n == 1:
                bank_rr[0] = (cand + 1) % 6
            return psum[:, cand * 512:(cand + n) * 512], ["ps%d" % (cand + j) for j in range(n)]

        def rel(keys):
            for k in keys:
                bank_free[int(k[2:])] = True

        ident_b = carve([128, 128], BF16)
        A2c = carve([128, 8, 2], F32)
        S2c = carve([128, 8, 2], F32)
        G2r = carve([128, 2, D], F32)
        persistB_end = off[0]
        cf = carve([128, NCP], F32)
        vr = carve([128, NVR], F32)
        vc = carve([128, NVC], F32)
        ones_b = carve([128, 128], BF16)
        mcur_b = carve([128, 128], BF16)
        mprev_b = carve([128, 128], BF16)
        cosT = carve([128, NT, 8], F32)
        sinT = carve([128, NT, 8], F32)
        esink = carve([128, 8], F32)
        A1c = carve([128, 8, 2], F32)
        S1c = carve([128, 8, 2], F32)
        G1r = carve([128, 2, D], F32)
        persist_end = off[0]

        tri_f = cf[:, C_TRI:C_TRI + 128]
        ones_f = cf[:, C_ONE:C_ONE + 128]
        maskw_f = cf[:, C_MW:C_MW + 128]

        P.dma("sp", lambda h: h.dma_start(out=cf, in_=cf_d[:, 0:NCP]), "cf", w=["cf"])
        P.dma("sp", lambda h: h.dma_start(out=vr, in_=vr_d), "vr", w=["vr"])
        P.dma("sp", lambda h: h.dma_start(out=vc, in_=vc_d), "vc", w=["vc"])
        P.op("act", lambda h: h.activation(out=esink, in_=vr[:, V_SINK:V_SINK + 8], func=AF.Exp), r=["vr"], w=["esink"])

        woff = [0]
        win_sb = carve([128, 8, INW], BF16, warena, woff)
        wao_sb = carve([128, 4, D], BF16, warena, woff)
        wmo_sb = carve([128, 4, D], BF16, warena, woff)
        wout_sb = carve([128, 8, D], BF16, warena, woff)

        cfs = carve([128, NCF - NCP], F32)
        P.dma("sp", lambda h: h.dma_start(out=cfs, in_=cf_d[:, NCP:NCF]), "cfs", w=["cfs"])
        P.op("dve", lambda h: h.tensor_copy(out=ident_b, in_=cfs[:, C_ID - NCP:C_ID - NCP + 128]), r=["cfs"], w=["ident_b"])
        P.op("dve", lambda h: h.tensor_copy(out=ones_b, in_=cf[:, C_ONE:C_ONE + 128]), r=["cf"], w=["ones_b"])
        P.op("dve", lambda h: h.tensor_copy(out=mcur_b, in_=cfs[:, C_MC - NCP:C_MC - NCP + 128]), r=["cfs"], w=["mcur_b"])
        P.op("dve", lambda h: h.tensor_copy(out=mprev_b, in_=cfs[:, C_MP - NCP:C_MP - NCP + 128]), r=["cfs"], w=["mprev_b"])
        posi = carve([128, NT], I32)
        posf = carve([128, NT], F32)
        ang = carve([128, NT, 8], F32)
        kk_i = carve([128, NT, 8], I32)
        kk_f = carve([128, NT, 8], F32)
        rr = carve([128, NT, 8], F32)
        rr2 = carve([128, NT, 8], F32)
        tmpa = carve([128, NT, 8], F32)
        ctf = carve([128, 8, 2], F32)
        ctb = carve([128, 8, 2], BF16)
        crep = carve([128, 8, 2, 128], BF16)
        wtail = woff[0]
        stage = [carve([128, 8, D], BF16, warena, woff), carve([128, 8, D], BF16)]
        vs = carve([128, NVS], F32)
        P.dma("sp", lambda h: h.dma_start(out=vs, in_=vs_d), "vs", w=["vs"])
        adat = carve([128, 8, 2], F32)
        gtmp = carve([128, D], F32)

        P.dma("sp", lambda h: h.dma_start(out=posi, in_=pos_d), "posi", w=["posi"])
        P.dma("sp", lambda h: h.dma_start(out=ctf, in_=ct_d), "ctf", w=["ctf"])
        ada_cols = [0, 1024, 2048, 3072, 4096, 5120]
        wada_v = wada_d.rearrange("(k p) n -> p k n", p=128)

        def load_stage(i):
            c0 = ada_cols[i]
            st = stage[i % 2]
            P.dma("pool", lambda h: h.dma_start(out=st, in_=wada_v[:, :, c0:c0 + D]), "stage%d" % (i % 2),
                  w=["stage%d" % (i % 2)])

        P.op("dve", lambda h: h.tensor_copy(out=posf, in_=posi), r=["posi"], w=["posf"])
        invf_b = cfs[:, C_INVF - NCP:C_INVF - NCP + 8].unsqueeze(1).to_broadcast([128, NT, 8])
        P.op("dve", lambda h: h.tensor_tensor(out=ang, in0=posf.unsqueeze(2).to_broadcast([128, NT, 8]), in1=invf_b,
                                              op=ALU.mult), r=["posf", "cfs"], w=["ang"])
        P.op("dve", lambda h: h.tensor_scalar(out=kk_f, in0=ang, scalar1=1.0 / (2 * PI), scalar2=None, op0=ALU.mult),
             r=["ang"], w=["kk_f"])
        P.op("dve", lambda h: h.tensor_copy(out=kk_i, in_=kk_f), r=["kk_f"], w=["kk_i"])
        P.op("dve", lambda h: h.tensor_copy(out=kk_f, in_=kk_i), r=["kk_i"], w=["kk_f"])
        P.op("dve", lambda h: h.scalar_tensor_tensor(out=rr, in0=kk_f, scalar=-2 * PI, in1=ang, op0=ALU.mult, op1=ALU.add),
             r=["kk_f", "ang"], w=["rr"])

        def wrap_sin(dst, shift, tag):
            P.op("dve", lambda h: h.tensor_scalar(out=rr2, in0=rr, scalar1=float(shift), scalar2=None, op0=ALU.add),
                 r=["rr"], w=["rr2"])
            P.op("dve", lambda h: h.tensor_scalar(out=tmpa, in0=rr2, scalar1=PI, scalar2=-2 * PI, op0=ALU.is_gt, op1=ALU.mult),
                 r=["rr2"], w=["tmpa"])
            P.op("dve", lambda h: h.tensor_tensor(out=rr2, in0=rr2, in1=tmpa, op=ALU.add), r=["rr2", "tmpa"], w=["rr2"])
            P.op("dve", lambda h: h.tensor_scalar(out=tmpa, in0=rr2, scalar1=-PI, scalar2=2 * PI, op0=ALU.is_lt, op1=ALU.mult),
                 r=["rr2"], w=["tmpa"])
            P.op("dve", lambda h: h.tensor_tensor(out=rr2, in0=rr2, in1=tmpa, op=ALU.add), r=["rr2", "tmpa"], w=["rr2"])
            P.op("act", lambda h: h.activation(out=dst, in_=rr2, func=AF.Sin), r=["rr2"], w=[tag])

        ck("consts")
        wrap_sin(sinT, 0.0, "sinT")
        wrap_sin(cosT, PI / 2, "cosT")

        ck("rope")
        P.op("act", lambda h: h.activation(out=ctb, in_=ctf, func=AF.Silu), r=["ctf"], w=["ctb"])
        for b in range(2):
            P.op("dve", lambda h, b=b: h.tensor_copy(out=crep[:, :, b, :], in_=ctb[:, :, b:b + 1].to_broadcast([128, 8, 128])),
                 r=["ctb"], w=["crep%d" % b])

        load_stage(0)
        load_stage(1)

        def ada_col(i, dst, bias_off, gain_off, plus_one):
            st = stage[i % 2]
            ps, pk = bank()
            for j in range(8):
                for k in range(8):
                    P.op("pe", lambda h, j=j, k=k: h.matmul(ps[:, j * 2:j * 2 + 2], lhsT=st[:, k, j * 128:(j + 1) * 128],
                                                            rhs=ctb[:, k, :], start=(k == 0), stop=(k == 7)),
                         r=["stage%d" % (i % 2), "ctb"], w=pk)
            psv = ps[:, 0:16].rearrange("p (j b) -> p j b", j=8)
            bia = vc[:, bias_off:bias_off + 8].unsqueeze(2).to_broadcast([128, 8, 2])
            P.op("dve", lambda h: h.tensor_tensor(out=adat, in0=psv, in1=bia, op=ALU.add), r=pk + ["vc"], w=["adat"])
            if plus_one:
                gn = vc[:, gain_off:gain_off + 8].unsqueeze(2).to_broadcast([128, 8, 2])
                P.op("dve", lambda h: h.scalar_tensor_tensor(out=dst, in0=adat, scalar=1.0, in1=gn, op0=ALU.add, op1=ALU.mult),
                     r=["adat", "vc"], w=["adacol%d" % i])
            else:
                P.op("dve", lambda h: h.tensor_copy(out=dst, in_=adat), r=["adat"], w=["adacol%d" % i])

        def ada_row(i, dst, bias_off, gain_off):
            st = stage[i % 2]
            for b in range(2):
                ps, pk = bank(2)
                for c in range(2):
                    for k in range(8):
                        P.op("pe", lambda h, b=b, c=c, k=k, ps=ps: h.matmul(ps[:, c * 512:(c + 1) * 512], lhsT=crep[:, k, b, :],
                                                                     rhs=st[:, k, c * 512:(c + 1) * 512],
                                                                     start=(k == 0), stop=(k == 7)),
                             r=["stage%d" % (i % 2), "crep%d" % b], w=[pk[c]])
                P.op("dve", lambda h, ps=ps: h.tensor_tensor(out=gtmp, in0=ps, in1=vs[:, bias_off:bias_off + D], op=ALU.add),
                     r=pk + ["vs"], w=["gtmp"])
                P.op("dve", lambda h, b=b: h.tensor_tensor(out=dst[:, b, :], in0=gtmp, in1=vs[:, gain_off:gain_off + D], op=ALU.mult),
                     r=["gtmp", "vs"], w=["adarow%d_%d" % (i, b)])

        ada_col(0, S1c, VC_BSH1, 0, False)
        load_stage(2)
        ada_col(1, A1c, VC_BSC1, VC_GPM, True)
        load_stage(3)
        win_v = win_d.rearrange("(k p) n -> p k n", p=128)
        for k in range(8):
            P.dma("pool", lambda h, k=k: h.dma_start(out=win_sb[:, k, :], in_=win_v[:, k, :]), "win%d" % k, w=["win%d" % k])
        ada_row(2, G1r, V_BG1, V_GPM)
        load_stage(4)
        ada_col(3, S2c, VC_BSH2, 0, False)
        load_stage(5)
        ada_col(4, A2c, VC_BSC2, VC_GPF, True)
        ada_row(5, G2r, V_BG2, V_GPF)
        wao_v = wao_d.rearrange("(k p) n -> p k n", p=128)
        wmo_v = wmo_d.rearrange("(k p) n -> p k n", p=128)
        wout_v = wout_d.rearrange("(k p) n -> p k n", p=128)
        P.dma("pool", lambda h: h.dma_start(out=wao_sb, in_=wao_v), "wao", w=["wao"])
        P.dma("pool", lambda h: h.dma_start(out=wmo_sb, in_=wmo_v), "wmo", w=["wmo"])
        P.dma("pool", lambda h: h.dma_start(out=wout_sb, in_=wout_v), "wout", w=["wout"])

        ck("setup")
        P.barrier()
        off[0] = persist_end
        WIN_KEYS = ["win%d" % k for k in range(8)]

        woff[0] = wtail
        X = [carve([128, D], F32, warena, woff) for _ in range(3)]
        x1t = carve([128, D], F32, warena, woff)
        t2 = x1t
        sigG = carve([128, 2048], F32, warena, woff)
        t1 = carve([128, D], F32, warena, woff)
        hT = [carve([128, 8, 128], BF16, warena, woff), carve([128, 8, 128], BF16)]
        kmT = [carve([64, 4, 128], BF16, warena, woff), carve([64, 4, 128], BF16)]
        junk = carve([128, 128], BF16)
        st8 = carve([128, 16], F32)
        xs = carve([128, D], BF16)
        qk_tm = carve([128, 10, 64], BF16)
        rt = carve([128, 4, 10, 8], F32)
        qkf = carve([128, 10, 64], F32)
        qaT = [carve([64, 8, 128], BF16) for _ in range(2)]
        kaT = [carve([64, 2, 128], BF16) for _ in range(3)]
        vaD = [carve([128, 2, 2, 64], BF16) for _ in range(3)]
        qm_tm = carve([128, 4, 64], BF16)
        km_tm = [carve([128, 4, 64], BF16) for _ in range(2)]
        qmT = [carve([64, 4, 128], BF16) for _ in range(2)]
        qsT = carve([64, 4, 128], BF16)
        vm1 = [carve([128, 4, 129], BF16) for _ in range(2)]
        sigO = [carve([128, 512], F32) for _ in range(2)]
        gs = [a_.rearrange("p (h d) -> p h d", h=4) for a_ in sigO]
        eT = [carve([128, 4, 128], BF16), carve([128, 4, 128], BF16)]
        pT = [carve([128, 4, 128], BF16), carve([128, 4, 128], BF16)]
        den2 = carve([128, 4, 128], F32)
        rec = den2
        attT = carve([128, 4, 128], BF16)
        gi = [carve([128, 8], F32) for _ in range(2)]
        th = carve([128, 8], F32)
        ef = carve([128, 4], F32)
        Fl = carve([128, 4], F32)
        imb = carve([128, 4], F32)
        Fm = carve([128, 4, 128], F32)
        Dexp = carve([128, 4, 128], F32)
        Eb = carve([64, 4, 128], F32)
        tmpW = Fm
        WT = carve([128, 4, 128], BF16)
        kw = carve([128, 4, 64], BF16)
        C1 = carve([64, 4, 129], F32)
        C1b = carve([64, 4, 129], BF16)
        dd = carve([128, 4], F32)
        ssq4 = carve([128, 4], F32)
        sm4 = carve([128, 4, 4], F32)
        ml = carve([128, 4, 128], BF16)
        mlT = carve([128, 4, 128], BF16)
        merged = carve([128, D], BF16)
        mT = carve([128, 8, 128], BF16)
        print("phaseA arena", off[0], "/", ABYTES, " warena", woff[0], "/", WBYTES)

        for i_ in range(2):
            P.op("pool", lambda h, i_=i_: h.memset(vm1[i_][:, :, 128:129], 1.0), w=["vm1_%d" % i_])

        def rsqrt_col(dst, src, scale, keys_r, key_w, tmpcol):
            P.op("act", lambda h: h.activation(out=tmpcol, in_=src, func=AF.Ln, scale=float(scale), bias=EPS),
                 r=keys_r, w=[key_w + "_sq"])
            P.op("act", lambda h: h.activation(out=dst, in_=tmpcol, func=AF.Exp, scale=-0.5), r=[key_w + "_sq"], w=[key_w])

        x_v = x_d.rearrange("(t p) n -> t p n", p=128)
        x1_v = x1_d.rearrange("(t p) n -> t p n", p=128)
        out_v = out_d.rearrange("(t p) n -> t p n", p=128)

        FL = {}

        def load_x(t):
            s = t % 3
            P.dma("sp", lambda h: h.dma_start(out=X[s], in_=x_v[t]), "X%d" % s, w=["X%d" % s])

        def make_tile(t):
            b = t // TPS
            n = t % TPS
            s = t % 3
            p = t % 2
            p3 = t % 3
            q3 = (t - 1) % 3
            Xk = "X%d" % s
            hT_, K_hT = hT[p], "hT%d" % p
            qaT_, K_qaT = qaT[p], "qaT%d" % p
            km_tm_, K_kmtm = km_tm[p], "km_tm%d" % p
            kmT_, K_kmT = kmT[p], "kmT%d" % p
            qmT_, K_qmT = qmT[p], "qmT%d" % p
            vm1_, K_vm1 = vm1[p], "vm1_%d" % p
            sigO_, gs_, K_sigO = sigO[p], gs[p], "sigO%d" % p
            gi_, K_gi = gi[p], "gi%d" % p

            def wait_flag(key):
                while not FL.get(key):
                    yield

            def proj(c0, n_, ps_ap, keys):
                for k in range(8):
                    P.op("pe", lambda h, k=k: h.matmul(ps_ap[:, 0:n_], lhsT=hT_[:, k, :], rhs=win_sb[:, k, c0:c0 + n_],
                                                       start=(k == 0), stop=(k == 7)),
                         r=[K_hT, "win%d" % k], w=keys, track_reads=True)

            def g_front():

                P.op("act", lambda h: h.activation(out=xs, in_=X[s], func=AF.Square, accum_out=st8[:, 0:1]),
                     r=[Xk], w=["xs", "ssq1"])
                yield
                rsqrt_col(st8[:, 2:3], st8[:, 0:1], 1.0 / D, ["ssq1"], "rstd1", st8[:, 1:2])
                yield
                P.op("dve", lambda h: h.tensor_scalar(out=xs, in0=X[s], scalar1=st8[:, 2:3], scalar2=None, op0=ALU.mult),
                     r=[Xk, "rstd1"], w=["xs"])
                yield
                ps, pk = yield from gbank()
                psb = ps.bitcast(BF16).rearrange("p (k n) -> p k n", k=8)
                for k in range(8):
                    P.op("pe", lambda h, k=k: h.transpose(out=psb[:, k, :], in_=xs[:, k * 128:(k + 1) * 128], identity=ident_b),
                         r=["xs", "ident_b"], w=pk)
                    yield
                for k in range(8):
                    P.op("act", lambda h, k=k: h.activation(out=hT_[:, k, :], in_=psb[:, k, :], func=AF.Identity,
                                                            bias=S1c[:, k, b:b + 1], scale=A1c[:, k, b:b + 1]),
                         r=pk + ["adacol0", "adacol1"], w=[K_hT])
                    yield
                rel(pk)
                ps_c, k_c = yield from gbank()
                proj(1024, 256, ps_c, k_c)
                yield
                proj(2304, 8, ps_c[:, 256:264], k_c)
                yield
                P.op("dve", lambda h: h.tensor_tensor(out=gi_, in0=ps_c[:, 256:264], in1=vr[:, V_BG:V_BG + 8], op=ALU.add),
                     r=k_c + ["vr"], w=[K_gi])
                yield
                FL[(t, "gi")] = True
                P.op("act", lambda h: h.copy(out=km_tm_, in_=ps_c[:, 0:256].rearrange("p (h d) -> p h d", h=4)), r=k_c, w=[K_kmtm])
                yield
                rel(k_c)
                ps_qa, k_qa = yield from gbank()
                proj(0, 512, ps_qa, k_qa)
                yield
                qa3 = ps_qa.rearrange("p (h d) -> p h d", h=8)
                P.op("act", lambda h: h.copy(out=qkf[:, 0:8, :], in_=qa3), r=k_qa, w=["qkf"])
                yield
                rel(k_qa)
                ps_b, k_b = yield from gbank()
                proj(512, 512, ps_b, k_b)
                yield
                ka3 = ps_b[:, 0:128].rearrange("p (h d) -> p h d", h=2)
                P.op("act", lambda h: h.copy(out=qkf[:, 8:10, :], in_=ka3), r=k_b, w=["qkf"])
                yield
                va3 = ps_b[:, 128:256].rearrange("p (h d) -> p h d", h=2)
                for dup in range(2):
                    P.op("dve", lambda h, dup=dup: h.tensor_copy(out=vaD[p3][:, :, dup, :], in_=va3), r=k_b, w=["vaD%d" % p3])
                    yield
                P.op("act", lambda h: h.copy(out=qm_tm, in_=ps_b[:, 256:512].rearrange("p (h d) -> p h d", h=4)), r=k_b, w=["qm_tm"])
                yield
                rel(k_b)
                P.op("dve", lambda h: h.tensor_copy(out=qk_tm, in_=qkf), r=["qkf"], w=["qk_tm"])
                yield
                ps_vm, k_vm = yield from gbank()
                proj(1280, 512, ps_vm, k_vm)
                yield
                P.op("dve", lambda h: h.tensor_copy(out=vm1_[:, :, 0:128], in_=ps_vm.rearrange("p (h d) -> p h d", h=4)),
                     r=k_vm, w=[K_vm1])
                yield
                rel(k_vm)
                ps_om, k_om = yield from gbank()
                proj(1792, 512, ps_om, k_om)
                yield
                P.op("act", lambda h: h.activation(out=sigO_, in_=ps_om, func=AF.Exp, scale=-1.0), r=k_om, w=[K_sigO])
                yield
                rel(k_om)
                P.op("act", lambda h: h.activation(out=sigO_, in_=sigO_, func=AF.Ln, bias=1.0), r=[K_sigO], w=[K_sigO])
                yield
                P.op("act", lambda h: h.activation(out=sigO_, in_=sigO_, func=AF.Exp, scale=-1.0), r=[K_sigO], w=[K_sigO])
                yield
                P.op("pool", lambda h: h.tensor_tensor(out=gs_, in0=sigO_.rearrange("p (h d) -> p h d", h=4),
                                                       in1=vr[:, V_GN:V_GN + 512].rearrange("p (h d) -> p h d", h=4), op=ALU.mult),
                     r=[K_sigO, "vr"], w=[K_sigO])
                yield
                cs = cosT[:, t, :].unsqueeze(1)
                sn = sinT[:, t, :].unsqueeze(1)
                for (src, h0, nh, kk) in ((qkf[:, 0:8, :], 0, 8, ["qkf"]), (qkf[:, 8:10, :], 8, 2, ["qkf"])):
                    cb = cs.to_broadcast([128, nh, 8])
                    sb = sn.to_broadcast([128, nh, 8])
                    x1_ = src[:, :, 0:8]
                    x2_ = src[:, :, 8:16]
                    r0 = rt[:, 0, h0:h0 + nh, :]
                    r1 = rt[:, 1, h0:h0 + nh, :]
                    r2 = rt[:, 2, h0:h0 + nh, :]
                    r3 = rt[:, 3, h0:h0 + nh, :]
                    tg = "rt%d" % h0
                    P.op("dve", lambda h, r0=r0, x1_=x1_, cb=cb: h.tensor_tensor(out=r0, in0=x1_, in1=cb, op=ALU.mult),
                         r=kk + ["cosT"], w=[tg + "a"])
                    yield
                    P.op("dve", lambda h, r1=r1, x2_=x2_, sb=sb: h.tensor_tensor(out=r1, in0=x2_, in1=sb, op=ALU.mult),
                         r=kk + ["sinT"], w=[tg + "b"])
                    yield
                    P.op("dve", lambda h, r2=r2, x2_=x2_, cb=cb: h.tensor_tensor(out=r2, in0=x2_, in1=cb, op=ALU.mult),
                         r=kk + ["cosT"], w=[tg + "c"])
                    yield
                    P.op("dve", lambda h, r3=r3, x1_=x1_, sb=sb: h.tensor_tensor(out=r3, in0=x1_, in1=sb, op=ALU.mult),
                         r=kk + ["sinT"], w=[tg + "d"])
                    yield
                    P.op("dve", lambda h, r0=r0, r1=r1, h0=h0, nh=nh: h.tensor_tensor(out=qk_tm[:, h0:h0 + nh, 0:8], in0=r0, in1=r1,
                                                                                    op=ALU.subtract),
                         r=[tg + "a", tg + "b"], w=["qk_tm"])
                    yield
                    P.op("dve", lambda h, r2=r2, r3=r3, h0=h0, nh=nh: h.tensor_tensor(out=qk_tm[:, h0:h0 + nh, 8:16], in0=r2, in1=r3,
                                                                                    op=ALU.add),
                         r=[tg + "c", tg + "d"], w=["qk_tm"])
                    yield
                ps1, k1 = yield from gbank()
                p1b = ps1.bitcast(BF16)
                qaT_ps = p1b[0:64, :].rearrange("p (h n) -> p h n", h=8)
                for hh in range(8):
                    P.op("pe", lambda h, hh=hh: h.transpose(out=qaT_ps[:, hh, :], in_=qk_tm[:, hh, :], identity=ident_b),
                         r=["qk_tm", "ident_b"], w=k1)
                    yield
                P.op("act", lambda h: h.copy(out=qaT_, in_=qaT_ps), r=k1, w=[K_qaT])
                yield
                rel(k1)
                ps2, k2 = yield from gbank()
                p2b = ps2.bitcast(BF16)
                kaT_ps = p2b[0:64, 0:256].rearrange("p (h n) -> p h n", h=2)
                kmT_ps = p2b[0:64, 256:768].rearrange("p (h n) -> p h n", h=4)
                for hh in range(2):
                    P.op("pe", lambda h, hh=hh: h.transpose(out=kaT_ps[:, hh, :], in_=qk_tm[:, 8 + hh, :], identity=ident_b),
                         r=["qk_tm", "ident_b"], w=k2)
                    yield
                for hh in range(4):
                    P.op("pe", lambda h, hh=hh: h.transpose(out=kmT_ps[:, hh, :], in_=km_tm_[:, hh, :], identity=ident_b),
                         r=[K_kmtm, "ident_b"], w=k2)
                    yield
                P.op("dve", lambda h: h.tensor_copy(out=kaT[p3], in_=kaT_ps), r=k2, w=["kaT%d" % p3])
                yield
                P.op("dve", lambda h: h.tensor_copy(out=kmT_, in_=kmT_ps), r=k2, w=[K_kmT])
                yield
                rel(k2)
                ps3, k3 = yield from gbank()
                p3b = ps3.bitcast(BF16)
                qmT_ps = p3b[0:64, 0:512].rearrange("p (h n) -> p h n", h=4)
                for hh in range(4):
                    P.op("pe", lambda h, hh=hh: h.transpose(out=qmT_ps[:, hh, :], in_=qm_tm[:, hh, :], identity=ident_b),
                         r=["qm_tm", "ident_b"], w=k3)
                    yield
                P.op("act", lambda h: h.copy(out=qmT_, in_=qmT_ps), r=k3, w=[K_qmT])
                yield
                rel(k3)


            def g_gates():

                for c in range(4):
                    if t > 0 and c == 0:
                        yield from wait_flag((t - 1, "t12_done"))
                    psg, kg = yield from gbank()
                    proj(2312 + c * 512, 512, psg, kg)
                    yield
                    sgc = sigG[:, c * 512:(c + 1) * 512]
                    P.op("act", lambda h, sgc=sgc, psg=psg: h.activation(out=sgc, in_=psg, func=AF.Exp, scale=-1.0),
                         r=kg, w=["sigG%d" % c])
                    yield
                    rel(kg)
                    P.op("act", lambda h, sgc=sgc: h.activation(out=sgc, in_=sgc, func=AF.Ln, bias=1.0), r=["sigG%d" % c], w=["sigG%d" % c])
                    yield
                    P.op("act", lambda h, sgc=sgc: h.activation(out=sgc, in_=sgc, func=AF.Exp, scale=-1.0), r=["sigG%d" % c], w=["sigG%d" % c])
                    yield


            def g_att(hk):

                blocks = [(p3, mcur_b, "mcur_b")]
                if n > 0:
                    blocks.append((q3, mprev_b, "mprev_b"))
                ps_o, k_o = yield from gbank()
                ps_d, k_d = yield from gbank()
                for bi, (slot, mask, mkey) in enumerate(blocks):
                    ps_s, k_s = yield from gbank()
                    P.op("pe", lambda h, slot=slot, hk=hk, ps_s=ps_s: h.matmul(
                        ps_s, lhsT=kaT[slot][:, hk, :], rhs=qaT_[:, 4 * hk:4 * hk + 4, :].rearrange("p a b -> p (a b)"), start=True, stop=True),
                        r=["kaT%d" % slot, K_qaT], w=k_s)
                    yield
                    P.op("act", lambda h, bi=bi, ps_s=ps_s: h.activation(out=eT[bi], in_=ps_s.rearrange("p (g n) -> p g n", g=4),
                                                                         func=AF.Exp, scale=0.125),
                         r=k_s, w=["eT%d" % bi])
                    yield
                    rel(k_s)
                    P.op("pool", lambda h, bi=bi, mask=mask: h.tensor_tensor(out=pT[bi], in0=eT[bi],
                                                                             in1=mask.unsqueeze(1).to_broadcast([128, 4, 128]),
                                                                             op=ALU.mult),
                         r=["eT%d" % bi, mkey], w=["pT%d" % bi])
                    yield
                    P.op("pe", lambda h, slot=slot, hk=hk, bi=bi, ps_o=ps_o: h.matmul(
                        ps_o, lhsT=vaD[slot][:, hk, :, :].rearrange("p a b -> p (a b)"), rhs=pT[bi].rearrange("p a b -> p (a b)"), start=(bi == 0), stop=(bi == len(blocks) - 1)),
                        r=["vaD%d" % slot, "pT%d" % bi], w=k_o)
                    yield
                    P.op("pe", lambda h, bi=bi, ps_d=ps_d: h.matmul(
                        ps_d, lhsT=ones_b, rhs=pT[bi].rearrange("p a b -> p (a b)"), start=(bi == 0), stop=(bi == len(blocks) - 1)),
                        r=["ones_b", "pT%d" % bi], w=k_d)
                    yield
                es4 = esink[:, 4 * hk:4 * hk + 4].unsqueeze(2).to_broadcast([128, 4, 128])
                P.op("dve", lambda h, ps_d=ps_d, es4=es4: h.tensor_tensor(out=den2, in0=ps_d.rearrange("p (g n) -> p g n", g=4),
                                                                         in1=es4, op=ALU.add),
                     r=k_d + ["esink"], w=["den2"])
                yield
                rel(k_d)
                P.op("act", lambda h: h.activation(out=rec, in_=den2, func=AF.Ln), r=["den2"], w=["den2"])
                yield
                P.op("act", lambda h: h.activation(out=rec, in_=rec, func=AF.Exp, scale=-1.0), r=["den2"], w=["den2"])
                yield
                if t > 0:
                    yield from wait_flag((t - 1, "a_done"))
                o4 = ps_o.rearrange("p (j g n) -> p j g n", j=2, g=2)
                r4 = rec.rearrange("p (j g) n -> p j g n", j=2)
                for half in range(2):
                    lo, hi = half * 64, half * 64 + 64
                    for j in range(2):
                        P.op("dve", lambda h, lo=lo, hi=hi, half=half, j=j, o4=o4, r4=r4, hk=hk: h.tensor_tensor(
                            out=attT[lo:hi, 2 * hk + j, :], in0=o4[lo:hi, j, half, :], in1=r4[lo:hi, j, half, :], op=ALU.mult),
                            r=k_o + ["den2"], w=["attT"])
                        yield
                rel(k_o)


            def g_mlg():
                yield from wait_flag((t, "gi"))
                if t > 0:
                    yield from wait_flag((t - 1, "dexp_done"))

                P.op("act", lambda h: h.activation(out=th, in_=gi_, func=AF.Exp, scale=2.0 / 15), r=[K_gi], w=["th"])
                yield
                P.op("dve", lambda h: h.tensor_scalar(out=th, in0=th, scalar1=1.0, scalar2=None, op0=ALU.add), r=["th"], w=["th"])
                yield
                P.op("dve", lambda h: h.reciprocal(out=th, in_=th), r=["th"], w=["th"])
                yield
                P.op("dve", lambda h: h.tensor_scalar(out=th, in0=th, scalar1=-2.0, scalar2=1.0, op0=ALU.mult, op1=ALU.add), r=["th"], w=["th"])
                yield
                P.op("act", lambda h: h.activation(out=ef, in_=th[:, 4:8], func=AF.Exp, scale=-15.0), r=["th"], w=["ef"])
                yield
                P.op("act", lambda h: h.activation(out=ef, in_=ef, func=AF.Ln, bias=1.0), r=["ef"], w=["ef"])
                yield
                P.op("dve", lambda h: h.tensor_scalar(out=Fl, in0=ef, scalar1=-1.0, scalar2=None, op0=ALU.mult), r=["ef"], w=["Fl"])
                yield
                ps_cs, k_cs = yield from gbank()
                P.op("pe", lambda h: h.matmul(ps_cs[:, 0:4], lhsT=tri_f, rhs=Fl, start=True, stop=True), r=["cf", "Fl"], w=k_cs)
                yield
                P.op("dve", lambda h: h.scalar_tensor_tensor(out=imb, in0=th[:, 0:4], scalar=15.0, in1=ps_cs[:, 0:4],
                                                             op0=ALU.mult, op1=ALU.subtract), r=["th"] + k_cs, w=["imb"])
                yield
                rel(k_cs)
                P.op("dve", lambda h: h.tensor_tensor(out=Fm, in0=tri_f.unsqueeze(1).to_broadcast([128, 4, 128]),
                                                      in1=Fl.unsqueeze(2).to_broadcast([128, 4, 128]), op=ALU.mult),
                     r=["cf", "Fl"], w=["Fm"])
                yield
                ps_rb, k_rb = yield from gbank()
                P.op("pe", lambda h: h.matmul(ps_rb, lhsT=ones_f, rhs=Fm.rearrange("p a b -> p (a b)"), start=True, stop=True), r=["cf", "Fm"], w=k_rb)
                yield
                rb3 = ps_rb.rearrange("p (g n) -> p g n", g=4)
                for hh in range(4):
                    P.op("act", lambda h, hh=hh: h.activation(out=Dexp[:, hh, :], in_=rb3[:, hh, :], func=AF.Exp, bias=imb[:, hh:hh + 1]),
                         r=k_rb + ["imb"], w=["Dexp"])
                    yield
                P.op("act", lambda h: h.activation(out=Eb, in_=rb3[0:64], func=AF.Exp), r=k_rb, w=["Eb"])
                yield
                rel(k_rb)

            def g_ml():
                P.op("dve", lambda h: h.scalar_tensor_tensor(out=qsT, in0=qmT_, scalar=0.125, in1=Eb, op0=ALU.mult, op1=ALU.mult),
                     r=[K_qmT, "Eb"], w=["qsT"])
                yield
                ps_st, k_st = yield from gbank()
                st3 = ps_st.rearrange("p (g n) -> p g n", g=4)
                for hh in range(4):
                    P.op("pe", lambda h, hh=hh: h.matmul(st3[:, hh, :], lhsT=kmT_[:, hh, :], rhs=qmT_[:, hh, :], start=True, stop=True),
                         r=[K_kmT, K_qmT], w=k_st)
                    yield
                P.op("pool", lambda h: h.tensor_tensor(out=tmpW, in0=Dexp, in1=maskw_f.unsqueeze(1).to_broadcast([128, 4, 128]),
                                                       op=ALU.mult), r=["Dexp", "cf"], w=["Fm"])
                yield
                P.op("dve", lambda h: h.tensor_tensor(out=WT, in0=tmpW, in1=st3, op=ALU.mult), r=["Fm"] + k_st, w=["WT"])
                yield
                rel(k_st)
                P.op("pool", lambda h: h.tensor_tensor(out=kw, in0=km_tm_, in1=Dexp[:, :, 127:128].to_broadcast([128, 4, 64]),
                                                       op=ALU.mult), r=[K_kmtm, "Dexp"], w=["kw"])
                yield
                nums = []
                for pr in range(2):
                    ps_n, k_n = yield from gbank()
                    n3 = ps_n.rearrange("p (g n) -> p g n", g=2)[:, :, 0:129]
                    for g in range(2):
                        hh = 2 * pr + g
                        P.op("pe", lambda h, hh=hh, g=g, n3=n3: h.matmul(n3[:, g, :], lhsT=WT[:, hh, :], rhs=vm1_[:, hh, :],
                                                                          start=True, stop=(n == 0)),
                             r=["WT", K_vm1], w=k_n)
                        yield
                        if n > 0:
                            P.op("pe", lambda h, hh=hh, g=g, n3=n3: h.matmul(n3[:, g, :], lhsT=qsT[:, hh, :], rhs=C1b[:, hh, :],
                                                                              start=False, stop=True),
                                 r=["qsT", "C1b"], w=k_n)
                            yield
                    nums.append((n3, k_n))
                cps = []
                for pr in range(2):
                    ps_c2, k_c2 = yield from gbank()
                    c3 = ps_c2[0:64, :].rearrange("p (g n) -> p g n", g=2)[:, :, 0:129]
                    for g in range(2):
                        hh = 2 * pr + g
                        P.op("pe", lambda h, hh=hh, g=g, c3=c3: h.matmul(c3[:, g, :], lhsT=kw[:, hh, :], rhs=vm1_[:, hh, :],
                                                                          start=True, stop=True),
                             r=["kw", K_vm1], w=k_c2)
                        yield
                    cps.append((c3, k_c2))
                if n > 0:
                    P.op("dve", lambda h: h.tensor_tensor(out=C1, in0=C1, in1=Eb[:, :, 127:128].to_broadcast([64, 4, 129]), op=ALU.mult),
                         r=["C1", "Eb"], w=["C1"])
                    yield
                    for pr in range(2):
                        c3, k_c2 = cps[pr]
                        for g in range(2):
                            P.op("dve", lambda h, pr=pr, g=g, c3=c3: h.tensor_tensor(out=C1[:, 2 * pr + g, :], in0=C1[:, 2 * pr + g, :],
                                                                                     in1=c3[:, g, :], op=ALU.add), r=["C1"] + k_c2, w=["C1"])
                            yield
                else:
                    for pr in range(2):
                        c3, k_c2 = cps[pr]
                        for g in range(2):
                            P.op("dve", lambda h, pr=pr, g=g, c3=c3: h.tensor_copy(out=C1[:, 2 * pr + g, :], in_=c3[:, g, :]), r=k_c2, w=["C1"])
                            yield
                FL[(t, "dexp_done")] = True
                P.op("pool", lambda h: h.tensor_copy(out=C1b, in_=C1), r=["C1"], w=["C1b"])
                yield
                for (_c3, _k) in cps:
                    rel(_k)
                for pr in range(2):
                    n3, k_n = nums[pr]
                    for g in range(2):
                        P.op("dve", lambda h, pr=pr, g=g, n3=n3: h.tensor_copy(out=dd[:, 2 * pr + g:2 * pr + g + 1], in_=n3[:, g, 128:129]),
                             r=k_n, w=["dd"])
                        yield
                    for g in range(2):
                        hh = 2 * pr + g
                        P.op("act", lambda h, hh=hh, g=g, n3=n3: h.activation(out=junk, in_=n3[:, g, 0:128], func=AF.Square,
                                                                              accum_out=ssq4[:, hh:hh + 1]),
                             r=k_n, w=["junk", "ssq4"])
                        yield
                sA = sm4[:, 0, :]
                sB = sm4[:, 1, :]
                sC = sm4[:, 2, :]
                sS = sm4[:, 3, :]
                P.op("dve", lambda h: h.scalar_tensor_tensor(out=dd, in0=dd, scalar=-1.0, in1=dd, op0=ALU.mult, op1=ALU.max), r=["dd"], w=["dd"])
                yield
                P.op("dve", lambda h: h.tensor_scalar(out=dd, in0=dd, scalar1=1.0, scalar2=None, op0=ALU.max), r=["dd"], w=["dd"])
                yield
                P.op("dve", lambda h: h.reciprocal(out=sA, in_=dd), r=["dd"], w=["sA"])
                yield
                P.op("dve", lambda h: h.tensor_tensor(out=sB, in0=sA, in1=sA, op=ALU.mult), r=["sA"], w=["sB"])
                yield
                P.op("dve", lambda h: h.tensor_tensor(out=sB, in0=sB, in1=ssq4, op=ALU.mult), r=["sB", "ssq4"], w=["sB"])
                yield
                P.op("act", lambda h: h.activation(out=sC, in_=sB, func=AF.Ln, scale=1.0 / 128, bias=EPS), r=["sB"], w=["sC"])
                yield
                P.op("act", lambda h: h.activation(out=sC, in_=sC, func=AF.Exp, scale=-0.5), r=["sC"], w=["sC"])
                yield
                P.op("dve", lambda h: h.tensor_tensor(out=sS, in0=sA, in1=sC, op=ALU.mult), r=["sA", "sC"], w=["sS"])
                yield
                for pr in range(2):
                    n3, k_n = nums[pr]
                    for g in range(2):
                        hh = 2 * pr + g
                        P.op("dve", lambda h, hh=hh, g=g, n3=n3: h.scalar_tensor_tensor(out=ml[:, hh, :], in0=n3[:, g, 0:128],
                                                                                        scalar=sm4[:, 3, hh:hh + 1], in1=gs_[:, hh, :],
                                                                                        op0=ALU.mult, op1=ALU.mult),
                             r=k_n + ["sS", K_sigO], w=["ml"])
                        yield
                for (_n3, _k) in nums:
                    rel(_k)
                ps_mt, k_mt = yield from gbank()
                mlT_ps = ps_mt.bitcast(BF16)[:, 0:512].rearrange("p (g n) -> p g n", g=4)
                for hh in range(4):
                    P.op("pe", lambda h, hh=hh: h.transpose(out=mlT_ps[:, hh, :], in_=ml[:, hh, :], identity=ident_b),
                         r=["ml", "ident_b"], w=k_mt)
                    yield
                if t > 0:
                    yield from wait_flag((t - 1, "mo_done"))
                P.op("act", lambda h: h.copy(out=mlT, in_=mlT_ps), r=k_mt, w=["mlT"])
                yield
                rel(k_mt)


            def g_tail():

                ps_a, k_a = yield from gbank(2)
                for c in range(2):
                    for j in range(4):
                        P.op("pe", lambda h, c=c, j=j: h.matmul(ps_a[:, c * 512:(c + 1) * 512], lhsT=attT[:, j, :],
                                                                rhs=wao_sb[:, j, c * 512:(c + 1) * 512], start=(j == 0), stop=(j == 3)),
                             r=["attT", "wao"], w=[k_a[c]])
                        yield
                FL[(t, "a_done")] = True
                P.op("dve", lambda h: h.tensor_tensor(out=t1, in0=ps_a, in1=sigG[:, 0:1024], op=ALU.mult),
                     r=k_a + ["sigG0", "sigG1"], w=["t1"])
                yield
                rel(k_a)
                ps_m, k_m = yield from gbank(2)
                for c in range(2):
                    for j in range(4):
                        P.op("pe", lambda h, c=c, j=j: h.matmul(ps_m[:, c * 512:(c + 1) * 512], lhsT=mlT[:, j, :],
                                                                rhs=wmo_sb[:, j, c * 512:(c + 1) * 512], start=(j == 0), stop=(j == 3)),
                             r=["mlT", "wmo"], w=[k_m[c]])
                        yield
                FL[(t, "mo_done")] = True
                P.op("dve", lambda h: h.tensor_tensor(out=t2, in0=ps_m, in1=sigG[:, 1024:2048], op=ALU.mult),
                     r=k_m + ["sigG2", "sigG3"], w=["x1t"])
                yield
                rel(k_m)
                FL[(t, "t12_done")] = True
                P.op("dve", lambda h: h.tensor_tensor(out=merged, in0=t1, in1=t2, op=ALU.add), r=["t1", "x1t"], w=["merged"])
                yield
                ps_t, k_t = yield from gbank()
                mT_ps = ps_t.bitcast(BF16).rearrange("p (k n) -> p k n", k=8)
                for k in range(8):
                    P.op("pe", lambda h, k=k: h.transpose(out=mT_ps[:, k, :], in_=merged[:, k * 128:(k + 1) * 128], identity=ident_b),
                         r=["merged", "ident_b"], w=k_t)
                    yield
                P.op("act", lambda h: h.copy(out=mT, in_=mT_ps), r=k_t, w=["mT"])
                yield
                rel(k_t)
                ps_x, k_x = yield from gbank(2)
                for c in range(2):
                    for k in range(8):
                        P.op("pe", lambda h, c=c, k=k: h.matmul(ps_x[:, c * 512:(c + 1) * 512], lhsT=mT[:, k, :],
                                                                rhs=wout_sb[:, k, c * 512:(c + 1) * 512], start=(k == 0), stop=(k == 7)),
                             r=["mT", "wout"], w=[k_x[c]])
                        yield
                P.op("act", lambda h: h.activation(out=merged, in_=ps_x, func=AF.Square, accum_out=st8[:, 4:5]), r=k_x, w=["merged", "ssq2"])
                yield
                rsqrt_col(st8[:, 6:7], st8[:, 4:5], 1.0 / D, ["ssq2"], "rstd2", st8[:, 5:6])
                yield
                P.op("dve", lambda h: h.scalar_tensor_tensor(out=t1, in0=ps_x, scalar=st8[:, 6:7], in1=G1r[:, b, :],
                                                             op0=ALU.mult, op1=ALU.mult),
                     r=k_x + ["rstd2", "adarow2_%d" % b], w=["t1"])
                yield
                rel(k_x)
                P.op("dve", lambda h: h.tensor_tensor(out=x1t, in0=X[s], in1=t1, op=ALU.add), r=[Xk, "t1"], w=["x1t"])
                yield
                P.dma("sp", lambda h: h.dma_start(out=x1_v[t], in_=x1t), "x1t", r=["x1t"], w=["x1d%d" % t],
                      final=(not do_ffn))
                yield


            return g_front, g_gates, g_att, g_ml, g_tail, g_mlg

        def interleave(gens):
            gens = list(gens)
            while gens:
                for g in list(gens):
                    try:
                        next(g)
                    except StopIteration:
                        gens.remove(g)

        tiles = [make_tile(t) for t in range(n_tiles)]

        def g_att_all(g_att):
            yield from g_att(0)
            yield from g_att(1)

        load_x(0)
        if n_tiles > 1:
            load_x(1)
        interleave([tiles[0][0](), tiles[0][5]()])
        for t in range(n_tiles):
            g_front, g_gates, g_att, g_ml, g_tail, g_mlg = tiles[t]
            th_ = []
            if t > 0:
                th_.append(tiles[t - 1][4]())
            th_ += [g_ml(), g_att_all(g_att), g_gates()]
            if t + 1 < n_tiles:
                th_ += [tiles[t + 1][0](), tiles[t + 1][5]()]
            interleave(th_)
            if t + 2 < n_tiles:
                load_x(t + 2)
        interleave([tiles[n_tiles - 1][4]()])

        if do_ffn:
            P.barrier()
            for i_ in range(8):
                bank_free[i_] = True
            off[0] = persistB_end
            woff[0] = 0
            wfi_sb = carve([128, 8, 2 * DFF], BF16, warena, woff)
            wfo_sb = carve([128, NF, D], BF16, warena, woff)
            wfi_v = wfi_d.rearrange("(k p) n -> p k n", p=128)
            wfo_v = wfo_d.rearrange("(k p) n -> p k n", p=128)
            FB = 6
            blocks_f = [(f0, min(NF, f0 + FB)) for f0 in range(0, NF, FB)]
            for bi_, (f0, f1) in enumerate(blocks_f):
                for nm, cofs in (("wfg", 0), ("wfu", DFF)):
                    P.dma("pool", lambda h, f0=f0, f1=f1, cofs=cofs: h.dma_start(
                        out=wfi_sb[:, :, cofs + f0 * 128:cofs + f1 * 128], in_=wfi_v[:, :, cofs + f0 * 128:cofs + f1 * 128]),
                        "%s%d" % (nm, bi_), w=["%s%d" % (nm, bi_)])
            for bi_, (f0, f1) in enumerate(blocks_f):
                P.dma("pool", lambda h, f0=f0, f1=f1: h.dma_start(out=wfo_sb[:, f0:f1, :], in_=wfo_v[:, f0:f1, :]),
                      "wfo%d" % bi_, w=["wfo%d" % bi_])

            GT = 4
            XB = [carve([128, D], F32) for _ in range(2)]
            sb8 = carve([128, 16], F32)
            xs2 = carve([128, D], BF16)
            h2T = [carve([128, 8, GT * 128], BF16) for _ in range(2)]
            actT = carve([128, NF, GT * 128], BF16)
            silu = [carve([128, GT * 128], F32), carve([128, GT * 128], F32)]
            junk2 = silu[0].bitcast(BF16)
            ot = [carve([128, D], F32), carve([128, D], F32)]
            rx = carve([128, D], F32)

            def g_prep(g_):
                b = (g_ * GT) // TPS
                hb = g_ % 2
                for j in range(GT):
                    t = g_ * GT + j
                    xb = XB[j % 2]
                    xk = "XB%d" % (j % 2)
                    P.dma("sp", lambda h, t=t, xb=xb: h.dma_start(out=xb, in_=x1_v[t]), xk, r=["x1d%d" % t], w=[xk])
                    yield
                    P.op("act", lambda h, xb=xb: h.activation(out=xs2, in_=xb, func=AF.Square, accum_out=sb8[:, 0:1]),
                         r=[xk], w=["xs2", "b_ssq"])
                    yield
                    rsqrt_col(sb8[:, 2:3], sb8[:, 0:1], 1.0 / D, ["b_ssq"], "b_rstd", sb8[:, 1:2])
                    yield
                    P.op("dve", lambda h, xb=xb: h.tensor_scalar(out=xs2, in0=xb, scalar1=sb8[:, 2:3], scalar2=None, op0=ALU.mult),
                         r=[xk, "b_rstd"], w=["xs2"])
                    yield
                    ps, pk = yield from gbank()
                    psb = ps.bitcast(BF16).rearrange("p (k n) -> p k n", k=8)
                    for k in range(8):
                        P.op("pe", lambda h, k=k, psb=psb: h.transpose(out=psb[:, k, :], in_=xs2[:, k * 128:(k + 1) * 128], identity=ident_b),
                             r=["xs2", "ident_b"], w=pk)
                        yield
                    for k in range(8):
                        P.op("act", lambda h, k=k, j=j, psb=psb, b=b, hb=hb: h.activation(
                            out=h2T[hb][:, k, j * 128:(j + 1) * 128], in_=psb[:, k, :], func=AF.Identity,
                            bias=S2c[:, k, b:b + 1], scale=A2c[:, k, b:b + 1]),
                            r=pk + ["adacol3", "adacol4"], w=["h2T%d" % hb])
                        yield
                    rel(pk)

            def g_ffn(g_):
                b = (g_ * GT) // TPS
                hb = g_ % 2
                hk_ = "h2T%d" % hb
                for f in range(NF):
                    blk = f // FB
                    ps_g, k_g = yield from gbank()
                    for k in range(8):
                        P.op("pe", lambda h, k=k, f=f, ps_g=ps_g: h.matmul(ps_g, lhsT=wfi_sb[:, k, f * 128:(f + 1) * 128],
                                                                           rhs=h2T[hb][:, k, :], start=(k == 0), stop=(k == 7)),
                             r=["wfg%d" % blk, hk_], w=k_g)
                    yield
                    ps_u, k_u = yield from gbank()
                    for k in range(8):
                        P.op("pe", lambda h, k=k, f=f, ps_u=ps_u: h.matmul(ps_u, lhsT=wfi_sb[:, k, DFF + f * 128:DFF + (f + 1) * 128],
                                                                           rhs=h2T[hb][:, k, :], start=(k == 0), stop=(k == 7)),
                             r=["wfu%d" % blk, hk_], w=k_u)
                    yield
                    sl = silu[f % 2]
                    P.op("act", lambda h, sl=sl, ps_g=ps_g: h.activation(out=sl, in_=ps_g, func=AF.Silu), r=k_g, w=["silu%d" % (f % 2)])
                    yield
                    rel(k_g)
                    P.op("dve", lambda h, sl=sl, f=f, ps_u=ps_u: h.tensor_tensor(out=actT[:, f, :], in0=sl, in1=ps_u, op=ALU.mult),
                         r=["silu%d" % (f % 2)] + k_u, w=["actT%d" % f])
                    yield
                    rel(k_u)
                for j in range(GT):
                    t = g_ * GT + j
                    ps_y, k_y = yield from gbank(2)
                    for c in range(2):
                        for f in range(NF):
                            P.op("pe", lambda h, c=c, f=f, j=j, ps_y=ps_y: h.matmul(
                                ps_y[:, c * 512:(c + 1) * 512], lhsT=actT[:, f, j * 128:(j + 1) * 128],
                                rhs=wfo_sb[:, f, c * 512:(c + 1) * 512], start=(f == 0), stop=(f == NF - 1)),
                                r=["actT%d" % f, "wfo%d" % (f // FB)], w=[k_y[c]])
                            if f % 4 == 3:
                                yield
                    P.dma("sp", lambda h, t=t: h.dma_start(out=rx, in_=x1_v[t]), "rx", r=["x1d%d" % t], w=["rx"])
                    yield
                    P.op("act", lambda h, ps_y=ps_y: h.activation(out=junk2, in_=ps_y, func=AF.Square, accum_out=sb8[:, 4:5]),
                         r=k_y, w=["silu0", "b_ssq2"])
                    yield
                    rsqrt_col(sb8[:, 6:7], sb8[:, 4:5], 1.0 / D, ["b_ssq2"], "b_rstd2", sb8[:, 5:6])
                    yield
                    o_ = ot[j % 2]
                    P.op("dve", lambda h, ps_y=ps_y, o_=o_, b=b: h.scalar_tensor_tensor(out=o_, in0=ps_y, scalar=sb8[:, 6:7], in1=G2r[:, b, :],
                                                                                   op0=ALU.mult, op1=ALU.mult),
                         r=k_y + ["b_rstd2", "adarow5_%d" % b], w=["ot%d" % (j % 2)])
                    yield
                    rel(k_y)
                    P.op("pool", lambda h, o_=o_: h.tensor_tensor(out=o_, in0=rx, in1=o_, op=ALU.add),
                         r=["rx", "ot%d" % (j % 2)], w=["ot%d" % (j % 2)])
                    yield
                    P.dma("sp", lambda h, o_=o_, t=t: h.dma_start(out=out_v[t], in_=o_), "ot%d" % (j % 2), r=["ot%d" % (j % 2)],
                          final=True)
                    yield

            n_groups = n_tiles // GT
            interleave([g_prep(0)])
            for g_ in range(n_groups):
                th_ = [g_ffn(g_)]
                if g_ + 1 < n_groups:
                    th_.append(g_prep(g_ + 1))
                interleave(th_)

        P.emit(nc, es)
    return nc


def _host_layout(inputs):
    f32 = np.float32
    x = np.asarray(inputs["x"], dtype=f32)
    c = np.asarray(inputs["c"], dtype=f32)
    pos = np.asarray(inputs["positions"], dtype=np.int32)
    cf = np.zeros((128, NCF), f32)
    ii = np.arange(128)
    cf[:, C_ID:C_ID + 128] = np.eye(128, dtype=f32)
    cf[:, C_TRI:C_TRI + 128] = (ii[:, None] <= ii[None, :]).astype(f32)
    cf[:, C_ONE:C_ONE + 128] = 1.0
    cf[:, C_MW:C_MW + 128] = 0.125 * (ii[:, None] <= ii[None, :]).astype(f32)
    cf[:, C_MC:C_MC + 128] = (ii[:, None] <= ii[None, :]).astype(f32)
    cf[:, C_MP:C_MP + 128] = (ii[:, None] > ii[None, :]).astype(f32)
    inv_freq = (np.float32(500000.0) ** (-np.arange(0, 16, 2, dtype=f32) / np.float32(16))).astype(f32)
    cf[:, C_INVF:C_INVF + 8] = inv_freq[None, :]

    def rep(v):
        return np.broadcast_to(np.asarray(v, f32).reshape(1, -1), (128, np.asarray(v).size))

    def col(v):
        return np.asarray(v, f32).reshape(-1, 128).T

    b_ada = np.asarray(inputs["b_ada"], f32)[0]
    vr = np.zeros((128, NVR), f32)
    vr[:, V_BG:V_BG + 8] = rep(inputs["b_mlstm_gates"][0])
    vr[:, V_SINK:V_SINK + 8] = rep(inputs["sinks"][0])
    vr[:, V_GN:V_GN + 512] = rep(inputs["g_mlstm_norm"][0])
    vs = np.zeros((128, NVS), f32)
    vs[:, V_GPM:V_GPM + D] = rep(inputs["g_post_mix"][0])
    vs[:, V_GPF:V_GPF + D] = rep(inputs["g_post_ffn"][0])
    vs[:, V_BG1:V_BG1 + D] = rep(b_ada[2048:3072])
    vs[:, V_BG2:V_BG2 + D] = rep(b_ada[5120:6144])
    vc = np.zeros((128, NVC), f32)
    vc[:, VC_GPM:VC_GPM + 8] = col(inputs["g_pre_mix"][0])
    vc[:, VC_GPF:VC_GPF + 8] = col(inputs["g_pre_ffn"][0])
    vc[:, VC_BSH1:VC_BSH1 + 8] = col(b_ada[0:1024])
    vc[:, VC_BSC1:VC_BSC1 + 8] = col(b_ada[1024:2048])
    vc[:, VC_BSH2:VC_BSH2 + 8] = col(b_ada[3072:4096])
    vc[:, VC_BSC2:VC_BSC2 + 8] = col(b_ada[4096:5120])
    shared = dict(
        cf=cf, vr=vr, vc=vc, vs=vs,
        w_ada=np.ascontiguousarray(inputs["w_ada"][0], dtype=f32),
        w_in=np.ascontiguousarray(inputs["w_in"][0], dtype=f32),
        w_att_o=np.ascontiguousarray(inputs["w_att_o"][0], dtype=f32),
        w_ml_o=np.ascontiguousarray(inputs["w_ml_o"][0], dtype=f32),
        w_out=np.ascontiguousarray(inputs["w_out"][0], dtype=f32),
        w_ffn_in=np.ascontiguousarray(inputs["w_ffn_in"][0], dtype=f32),
        w_ffn_out=np.ascontiguousarray(inputs["w_ffn_out"][0], dtype=f32),
    )
    in_maps = []
    for i in range(NCORES):
        xb = x[2 * i:2 * i + 2].reshape(2 * SEQ, D)
        pb = pos[2 * i:2 * i + 2].reshape(NT, 128).T
        cb = c[2 * i:2 * i + 2]
        ct = cb.reshape(2, 8, 128).transpose(2, 1, 0)
        m = dict(shared)
        m["x"] = np.ascontiguousarray(xb)
        m["pos"] = np.ascontiguousarray(pb, dtype=np.int32)
        m["ct"] = np.ascontiguousarray(ct, dtype=f32)
        in_maps.append(m)
    return in_maps


_NC_CACHE = {}


def kernel(**inputs):
    in_maps = _host_layout(inputs)
    if "nc" not in _NC_CACHE:
        _NC_CACHE["nc"] = build_nc()
    nc = _NC_CACHE["nc"]
    res = run_bass_kernel_spmd(nc, in_maps, core_ids=list(range(NCORES)))
    outs = [np.asarray(r["out"], dtype=np.float32).reshape(2, SEQ, D) for r in res.results]
    return np.concatenate(outs, axis=0)
```

```python
import numpy as np
from contextlib import ExitStack
import concourse.bass as bass
import concourse.mybir as mybir
from concourse.bass_utils import run_bass_kernel_spmd

F32 = mybir.dt.float32
BF16 = mybir.dt.bfloat16
I32 = mybir.dt.int32
U8 = mybir.dt.uint8
AF = mybir.ActivationFunctionType
ALU = mybir.AluOpType

ENGS = ("pe", "act", "dve", "pool", "sp")

import os
MLG_EARLY = os.environ.get('K_MLG_EARLY', '1') == '1'
NCORES = 8
SEQ = 2048
D = 1024
NT = 32
TPS = 16
INW = 4360
DFF = 2816
NF = 22
EPS = 1e-6
PI = float(np.pi)


class Op:
    __slots__ = ("eng", "fn", "deps", "needs_inc", "count", "is_dma", "slot", "val")

    def __init__(self, eng, fn):
        self.eng = eng
        self.fn = fn
        self.deps = []
        self.needs_inc = False
        self.count = 0
        self.is_dma = False
        self.slot = None
        self.val = 0


class Prog:
    def __init__(self):
        self.ops = {e: [] for e in ENGS}
        self.last_w = {}
        self.reads = {}
        self.slot_cnt = {}
        self.pending = {e: [] for e in ENGS}
        self.all_dma = []
        self.final_waits = []
        self.stopped = False

    def _add(self, eng, fn, r, w, track_reads=True):
        o = Op(eng, fn)
        if self.stopped:
            return o
        deps = []
        pr = [k for k in r if k.startswith("ps") and k[2:].isdigit()]
        if pr:
            r = [k for k in r if k not in pr]
            w = list(w) + [k for k in pr if k not in w]
        for k in r:
            lw = self.last_w.get(k)
            if lw is not None:
                deps.append(lw)
        for k in w:
            lw = self.last_w.get(k)
            if lw is not None:
                deps.append(lw)
            deps.extend(self.reads.get(k, ()))
        deps.extend(self.pending[eng])
        self.pending[eng] = []
        seen = set()
        for d in deps:
            if id(d) in seen:
                continue
            seen.add(id(d))
            if eng == "pe" and d.eng == "pe" and not d.is_dma:
                continue
            o.deps.append(d)
        if track_reads:
            for k in r:
                self.reads.setdefault(k, []).append(o)
        for k in w:
            self.last_w[k] = o
            self.reads[k] = []
        self.ops[eng].append(o)
        return o

    def op(self, eng, fn, r=(), w=(), track_reads=True):
        return self._add(eng, fn, r, w, track_reads)

    def dma(self, queue, fn, slot, r=(), w=(), final=False):
        if self.stopped:
            return None
        o = self._add(queue, fn, r, w)
        o.is_dma = True
        o.slot = slot
        self.slot_cnt[slot] = self.slot_cnt.get(slot, 0) + 1
        o.val = 16 * self.slot_cnt[slot]
        self.all_dma.append(o)
        if final:
            self.final_waits.append(o)
        return o

    def barrier(self):
        lasts = [self.ops[e][-1] for e in ENGS if self.ops[e]]
        lasts = [o for o in lasts if not o.is_dma]
        dmas = list(self.all_dma)
        self.all_dma = []
        for e in ENGS:
            self.pending[e] = self.pending[e] + lasts + dmas

    def emit(self, nc, es):
        for e in ENGS:
            for o in self.ops[e]:
                for d in o.deps:
                    if not d.is_dma:
                        d.needs_inc = True
        esem = {e: es.enter_context(nc.semaphore("s_" + e)) for e in ENGS}
        ssem = {s: es.enter_context(nc.semaphore("d_%d" % i)) for i, s in enumerate(self.slot_cnt)}
        for e in ENGS:
            c = 0
            for o in self.ops[e]:
                if (not o.is_dma) and o.needs_inc:
                    c += 1
                    o.count = c
        ops = self.ops
        final_waits = self.final_waits

        def token(d):
            if d.is_dma:
                return ssem[d.slot], d.val
            return esem[d.eng], d.count

        def replay(e, h):
            waited = {}
            for o in ops[e]:
                for d in o.deps:
                    s, v = token(d)
                    if waited.get(id(s), 0) < v:
                        h.wait_ge(s, v)
                        waited[id(s)] = v
                ins = o.fn(h)
                if o.is_dma:
                    ins.then_inc(ssem[o.slot], 16)
                elif o.needs_inc:
                    ins.then_inc(esem[e], 1)
            if e == "sp":
                for d in final_waits:
                    s, v = token(d)
                    if waited.get(id(s), 0) < v:
                        h.wait_ge(s, v)
                        waited[id(s)] = v

        with nc.Block() as block:
            @block.tensor
            def _(h):
                replay("pe", h)

            @block.scalar
            def _(h):
                replay("act", h)

            @block.vector
            def _(h):
                replay("dve", h)

            @block.gpsimd
            def _(h):
                replay("pool", h)

            @block.sync
            def _(h):
                replay("sp", h)


DTSIZE = {F32: 4, BF16: 2, I32: 4, U8: 1}

C_TRI, C_ONE, C_MW, C_ID, C_MC, C_MP, C_INVF = 0, 128, 256, 384, 512, 640, 768
NCP = 384
NCF = 776
V_BG, V_SINK, V_GN = 0, 8, 16
NVR = 528
V_GPM, V_GPF, V_BG1, V_BG2 = 0, 1024, 2048, 3072
NVS = 4096
VC_GPM, VC_GPF, VC_BSH1, VC_BSC1, VC_BSH2, VC_BSC2 = 0, 8, 16, 24, 32, 40
NVC = 48


def build_nc(n_tiles=NT, do_ffn=True, dbg=False, stop_at=None):
    nc = bass.Bass("TRN2", target_bir_lowering=False)
    ntok = NT * 128
    x_d = nc.dram_tensor("x", [ntok, D], F32, kind="ExternalInput").ap()
    pos_d = nc.dram_tensor("pos", [128, NT], I32, kind="ExternalInput").ap()
    ct_d = nc.dram_tensor("ct", [128, 8, 2], F32, kind="ExternalInput").ap()
    cf_d = nc.dram_tensor("cf", [128, NCF], F32, kind="ExternalInput").ap()
    vr_d = nc.dram_tensor("vr", [128, NVR], F32, kind="ExternalInput").ap()
    vs_d = nc.dram_tensor("vs", [128, NVS], F32, kind="ExternalInput").ap()
    vc_d = nc.dram_tensor("vc", [128, NVC], F32, kind="ExternalInput").ap()
    wada_d = nc.dram_tensor("w_ada", [D, 6 * D], F32, kind="ExternalInput").ap()
    win_d = nc.dram_tensor("w_in", [D, INW], F32, kind="ExternalInput").ap()
    wao_d = nc.dram_tensor("w_att_o", [512, D], F32, kind="ExternalInput").ap()
    wmo_d = nc.dram_tensor("w_ml_o", [512, D], F32, kind="ExternalInput").ap()
    wout_d = nc.dram_tensor("w_out", [D, D], F32, kind="ExternalInput").ap()
    wfi_d = nc.dram_tensor("w_ffn_in", [D, 2 * DFF], F32, kind="ExternalInput").ap()
    wfo_d = nc.dram_tensor("w_ffn_out", [DFF, D], F32, kind="ExternalInput").ap()
    out_d = nc.dram_tensor("out", [ntok, D], F32, kind="ExternalOutput").ap()
    x1_d = nc.dram_tensor("x1s", [ntok, D], F32, kind="Internal").ap()

    es = ExitStack()
    with es:
        WBYTES = (8 * 2 * DFF + NF * D) * 2
        ABYTES = 212863 - WBYTES - 64
        ABYTES = ABYTES // 32 * 32
        warena = es.enter_context(nc.sbuf_tensor("warena", [128, WBYTES], U8))
        arena = es.enter_context(nc.sbuf_tensor("arena", [128, ABYTES], U8))
        psum = es.enter_context(nc.psum_tensor("psum", [128, 4096], F32))
        off = [0]

        def carve(shape, dt, ar=None, offv=None):
            a_ = arena if ar is None else ar
            o_ = off if offv is None else offv
            n = int(np.prod(shape[1:])) * DTSIZE[dt]
            assert o_[0] + n <= (ABYTES if ar is None else WBYTES), (o_[0], n)
            a = a_[0:shape[0], o_[0]:o_[0] + n].bitcast(dt)
            o_[0] += (n + 31) // 32 * 32
            if len(shape) == 3:
                a = a.rearrange("p (a b) -> p a b", a=shape[1])
            elif len(shape) == 4:
                a = a.rearrange("p (a b c) -> p a b c", a=shape[1], b=shape[2])
            return a

        P = Prog()

        def ck(name):
            if stop_at is not None and name == stop_at:
                P.dma("sp", lambda h: h.dma_start(out=out_d[0:128, 0:16], in_=arena[:, 0:64].bitcast(F32)), "ckout", final=True)
                P.stopped = True

        bank_ctr = [0]

        def bank(n=1):
            b = bank_ctr[0]
            if n == 2 and b % 2 == 1:
                b += 1
            if b + n > 8:
                b = 0
            bank_ctr[0] = (b + n) % 8
            keys = ["ps%d" % (b + i) for i in range(n)]
            return psum[:, b * 512:(b + n) * 512], keys

        bank_free = [True] * 8
        bank_rr = [0]
        spin = [0]

        def gbank(n=1):
            while True:
                cand = None
                if n == 2:
                    if bank_free[6] and bank_free[7]:
                        cand = 6
                else:
                    for d in range(6):
                        i = (bank_rr[0] + d) % 6
                        if bank_free[i]:
                            cand = i
                            break
                if cand is not None:
                    break
                spin[0] += 1
                assert spin[0] < 200000, "PSUM allocator deadlock"
                yield
            spin[0] = 0
            for j in range(n):
                bank_free[cand + j] = False
            if n == 1:
                bank_rr[0] = (cand + 1) % 6
            return psum[:, cand * 512:(cand + n) * 512], ["ps%d" % (cand + j) for j in range(n)]

        def rel(keys):
            for k in keys:
                bank_free[int(k[2:])] = True

        ident_b = carve([128, 128], BF16)
        A2c = carve([128, 8, 2], F32)
        S2c = carve([128, 8, 2], F32)
        G2r = carve([128, 2, D], F32)
        persistB_end = off[0]
        cf = carve([128, NCP], F32)
        vr = carve([128, NVR], F32)
        vc = carve([128, NVC], F32)
        ones_b = carve([128, 128], BF16)
        mcur_b = carve([128, 128], BF16)
        mprev_b = carve([128, 128], BF16)
        cosT = carve([128, NT, 8], F32)
        sinT = carve([128, NT, 8], F32)
        esink = carve([128, 8], F32)
        A1c = carve([128, 8, 2], F32)
        S1c = carve([128, 8, 2], F32)
        G1r = carve([128, 2, D], F32)
        persist_end = off[0]

        tri_f = cf[:, C_TRI:C_TRI + 128]
        ones_f = cf[:, C_ONE:C_ONE + 128]
        maskw_f = cf[:, C_MW:C_MW + 128]

        P.dma("sp", lambda h: h.dma_start(out=cf, in_=cf_d[:, 0:NCP]), "cf", w=["cf"])
        P.dma("sp", lambda h: h.dma_start(out=vr, in_=vr_d), "vr", w=["vr"])
        P.dma("sp", lambda h: h.dma_start(out=vc, in_=vc_d), "vc", w=["vc"])
        P.op("act", lambda h: h.activation(out=esink, in_=vr[:, V_SINK:V_SINK + 8], func=AF.Exp), r=["vr"], w=["esink"])

        woff = [0]
        win_sb = carve([128, 8, INW], BF16, warena, woff)
        wao_sb = carve([128, 4, D], BF16, warena, woff)
        wmo_sb = carve([128, 4, D], BF16, warena, woff)
        wout_sb = carve([128, 8, D], BF16, warena, woff)

        cfs = carve([128, NCF - NCP], F32)
        P.dma("sp", lambda h: h.dma_start(out=cfs, in_=cf_d[:, NCP:NCF]), "cfs", w=["cfs"])
        P.op("dve", lambda h: h.tensor_copy(out=ident_b, in_=cfs[:, C_ID - NCP:C_ID - NCP + 128]), r=["cfs"], w=["ident_b"])
        P.op("dve", lambda h: h.tensor_copy(out=ones_b, in_=cf[:, C_ONE:C_ONE + 128]), r=["cf"], w=["ones_b"])
        P.op("dve", lambda h: h.tensor_copy(out=mcur_b, in_=cfs[:, C_MC - NCP:C_MC - NCP + 128]), r=["cfs"], w=["mcur_b"])
        P.op("dve", lambda h: h.tensor_copy(out=mprev_b, in_=cfs[:, C_MP - NCP:C_MP - NCP + 128]), r=["cfs"], w=["mprev_b"])
        posi = carve([128, NT], I32)
        posf = carve([128, NT], F32)
        ang = carve([128, NT, 8], F32)
        kk_i = carve([128, NT, 8], I32)
        kk_f = carve([128, NT, 8], F32)
        rr = carve([128, NT, 8], F32)
        rr2 = carve([128, NT, 8], F32)
        tmpa = carve([128, NT, 8], F32)
        ctf = carve([128, 8, 2], F32)
        ctb = carve([128, 8, 2], BF16)
        crep = carve([128, 8, 2, 128], BF16)
        wtail = woff[0]
        stage = [carve([128, 8, D], BF16, warena, woff), carve([128, 8, D], BF16)]
        vs = carve([128, NVS], F32)
        P.dma("sp", lambda h: h.dma_start(out=vs, in_=vs_d), "vs", w=["vs"])
        adat = carve([128, 8, 2], F32)
        gtmp = carve([128, D], F32)

        P.dma("sp", lambda h: h.dma_start(out=posi, in_=pos_d), "posi", w=["posi"])
        P.dma("sp", lambda h: h.dma_start(out=ctf, in_=ct_d), "ctf", w=["ctf"])
        ada_cols = [0, 1024, 2048, 3072, 4096, 5120]
        wada_v = wada_d.rearrange("(k p) n -> p k n", p=128)

        def load_stage(i):
            c0 = ada_cols[i]
            st = stage[i % 2]
            P.dma("pool", lambda h: h.dma_start(out=st, in_=wada_v[:, :, c0:c0 + D]), "stage%d" % (i % 2),
                  w=["stage%d" % (i % 2)])

        P.op("dve", lambda h: h.tensor_copy(out=posf, in_=posi), r=["posi"], w=["posf"])
        invf_b = cfs[:, C_INVF - NCP:C_INVF - NCP + 8].unsqueeze(1).to_broadcast([128, NT, 8])
        P.op("dve", lambda h: h.tensor_tensor(out=ang, in0=posf.unsqueeze(2).to_broadcast([128, NT, 8]), in1=invf_b,
                                              op=ALU.mult), r=["posf", "cfs"], w=["ang"])
        P.op("dve", lambda h: h.tensor_scalar(out=kk_f, in0=ang, scalar1=1.0 / (2 * PI), scalar2=None, op0=ALU.mult),
             r=["ang"], w=["kk_f"])
        P.op("dve", lambda h: h.tensor_copy(out=kk_i, in_=kk_f), r=["kk_f"], w=["kk_i"])
        P.op("dve", lambda h: h.tensor_copy(out=kk_f, in_=kk_i), r=["kk_i"], w=["kk_f"])
        P.op("dve", lambda h: h.scalar_tensor_tensor(out=rr, in0=kk_f, scalar=-2 * PI, in1=ang, op0=ALU.mult, op1=ALU.add),
             r=["kk_f", "ang"], w=["rr"])

        def wrap_sin(dst, shift, tag):
            P.op("dve", lambda h: h.tensor_scalar(out=rr2, in0=rr, scalar1=float(shift), scalar2=None, op0=ALU.add),
                 r=["rr"], w=["rr2"])
            P.op("dve", lambda h: h.tensor_scalar(out=tmpa, in0=rr2, scalar1=PI, scalar2=-2 * PI, op0=ALU.is_gt, op1=ALU.mult),
                 r=["rr2"], w=["tmpa"])
            P.op("dve", lambda h: h.tensor_tensor(out=rr2, in0=rr2, in1=tmpa, op=ALU.add), r=["rr2", "tmpa"], w=["rr2"])
            P.op("dve", lambda h: h.tensor_scalar(out=tmpa, in0=rr2, scalar1=-PI, scalar2=2 * PI, op0=ALU.is_lt, op1=ALU.mult),
                 r=["rr2"], w=["tmpa"])
            P.op("dve", lambda h: h.tensor_tensor(out=rr2, in0=rr2, in1=tmpa, op=ALU.add), r=["rr2", "tmpa"], w=["rr2"])
            P.op("act", lambda h: h.activation(out=dst, in_=rr2, func=AF.Sin), r=["rr2"], w=[tag])

        ck("consts")
        wrap_sin(sinT, 0.0, "sinT")
        wrap_sin(cosT, PI / 2, "cosT")

        ck("rope")
        P.op("act", lambda h: h.activation(out=ctb, in_=ctf, func=AF.Silu), r=["ctf"], w=["ctb"])
        for b in range(2):
            P.op("dve", lambda h, b=b: h.tensor_copy(out=crep[:, :, b, :], in_=ctb[:, :, b:b + 1].to_broadcast([128, 8, 128])),
                 r=["ctb"], w=["crep%d" % b])

        load_stage(0)
        load_stage(1)

        def ada_col(i, dst, bias_off, gain_off, plus_one):
            st = stage[i % 2]
            ps, pk = bank()
            for j in range(8):
                for k in range(8):
                    P.op("pe", lambda h, j=j, k=k: h.matmul(ps[:, j * 2:j * 2 + 2], lhsT=st[:, k, j * 128:(j + 1) * 128],
                                                            rhs=ctb[:, k, :], start=(k == 0), stop=(k == 7)),
                         r=["stage%d" % (i % 2), "ctb"], w=pk)
            psv = ps[:, 0:16].rearrange("p (j b) -> p j b", j=8)
            bia = vc[:, bias_off:bias_off + 8].unsqueeze(2).to_broadcast([128, 8, 2])
            P.op("dve", lambda h: h.tensor_tensor(out=adat, in0=psv, in1=bia, op=ALU.add), r=pk + ["vc"], w=["adat"])
            if plus_one:
                gn = vc[:, gain_off:gain_off + 8].unsqueeze(2).to_broadcast([128, 8, 2])
                P.op("dve", lambda h: h.scalar_tensor_tensor(out=dst, in0=adat, scalar=1.0, in1=gn, op0=ALU.add, op1=ALU.mult),
                     r=["adat", "vc"], w=["adacol%d" % i])
            else:
                P.op("dve", lambda h: h.tensor_copy(out=dst, in_=adat), r=["adat"], w=["adacol%d" % i])

        def ada_row(i, dst, bias_off, gain_off):
            st = stage[i % 2]
            for b in range(2):
                ps, pk = bank(2)
                for c in range(2):
                    for k in range(8):
                        P.op("pe", lambda h, b=b, c=c, k=k, ps=ps: h.matmul(ps[:, c * 512:(c + 1) * 512], lhsT=crep[:, k, b, :],
                                                                     rhs=st[:, k, c * 512:(c + 1) * 512],
                                                                     start=(k == 0), stop=(k == 7)),
                             r=["stage%d" % (i % 2), "crep%d" % b], w=[pk[c]])
                P.op("dve", lambda h, ps=ps: h.tensor_tensor(out=gtmp, in0=ps, in1=vs[:, bias_off:bias_off + D], op=ALU.add),
                     r=pk + ["vs"], w=["gtmp"])
                P.op("dve", lambda h, b=b: h.tensor_tensor(out=dst[:, b, :], in0=gtmp, in1=vs[:, gain_off:gain_off + D], op=ALU.mult),
                     r=["gtmp", "vs"], w=["adarow%d_%d" % (i, b)])

        ada_col(0, S1c, VC_BSH1, 0, False)
        load_stage(2)
        ada_col(1, A1c, VC_BSC1, VC_GPM, True)
        load_stage(3)
        win_v = win_d.rearrange("(k p) n -> p k n", p=128)
        for k in range(8):
            P.dma("pool", lambda h, k=k: h.dma_start(out=win_sb[:, k, :], in_=win_v[:, k, :]), "win%d" % k, w=["win%d" % k])
        ada_row(2, G1r, V_BG1, V_GPM)
        load_stage(4)
        ada_col(3, S2c, VC_BSH2, 0, False)
        load_stage(5)
        ada_col(4, A2c, VC_BSC2, VC_GPF, True)
        ada_row(5, G2r, V_BG2, V_GPF)
        wao_v = wao_d.rearrange("(k p) n -> p k n", p=128)
        wmo_v = wmo_d.rearrange("(k p) n -> p k n", p=128)
        wout_v = wout_d.rearrange("(k p) n -> p k n", p=128)
        P.dma("pool", lambda h: h.dma_start(out=wao_sb, in_=wao_v), "wao", w=["wao"])
        P.dma("pool", lambda h: h.dma_start(out=wmo_sb, in_=wmo_v), "wmo", w=["wmo"])
        P.dma("pool", lambda h: h.dma_start(out=wout_sb, in_=wout_v), "wout", w=["wout"])

        ck("setup")
        P.barrier()
        off[0] = persist_end
        WIN_KEYS = ["win%d" % k for k in range(8)]

        woff[0] = wtail
        X = [carve([128, D], F32, warena, woff) for _ in range(3)]
        x1t = carve([128, D], F32, warena, woff)
        t2 = x1t
        sigG = carve([128, 2048], F32, warena, woff)
        t1 = carve([128, D], F32, warena, woff)
        hT = [carve([128, 8, 128], BF16, warena, woff), carve([128, 8, 128], BF16)]
        kmT = [carve([64, 4, 128], BF16, warena, woff), carve([64, 4, 128], BF16)]
        junk = carve([128, 128], BF16)
        st8 = carve([128, 16], F32)
        xs = carve([128, D], BF16)
        qk_tm = carve([128, 10, 64], BF16)
        rt = carve([128, 4, 10, 8], F32)
        qkf = carve([128, 10, 64], F32)
        qaT = [carve([64, 8, 128], BF16) for _ in range(2)]
        kaT = [carve([64, 2, 128], BF16) for _ in range(3)]
        vaD = [carve([128, 2, 2, 64], BF16) for _ in range(3)]
        qm_tm = carve([128, 4, 64], BF16)
        km_tm = [carve([128, 4, 64], BF16) for _ in range(2)]
        qmT = [carve([64, 4, 128], BF16) for _ in range(2)]
        qsT = carve([64, 4, 128], BF16)
        vm1 = [carve([128, 4, 129], BF16) for _ in range(2)]
        sigO = [carve([128, 512], F32) for _ in range(2)]
        gs = [a_.rearrange("p (h d) -> p h d", h=4) for a_ in sigO]
        eT = [carve([128, 4, 128], BF16), carve([128, 4, 128], BF16)]
        pT = [carve([128, 4, 128], BF16), carve([128, 4, 128], BF16)]
        den2 = carve([128, 4, 128], F32)
        rec = den2
        attT = carve([128, 4, 128], BF16)
        gi = [carve([128, 8], F32) for _ in range(2)]
        th = carve([128, 8], F32)
        ef = carve([128, 4], F32)
        Fl = carve([128, 4], F32)
        imb = carve([128, 4], F32)
        Fm = carve([128, 4, 128], F32)
        Dexp = carve([128, 4, 128], F32)
        Eb = carve([64, 4, 128], F32)
        tmpW = Fm
        WT = carve([128, 4, 128], BF16)
        kw = carve([128, 4, 64], BF16)
        C1 = carve([64, 4, 129], F32)
        C1b = carve([64, 4, 129], BF16)
        dd = carve([128, 4], F32)
        ssq4 = carve([128, 4], F32)
        sm4 = carve([128, 4, 4], F32)
        ml = carve([128, 4, 128], BF16)
        mlT = carve([128, 4, 128], BF16)
        merged = carve([128, D], BF16)
        mT = carve([128, 8, 128], BF16)
        print("phaseA arena", off[0], "/", ABYTES, " warena", woff[0], "/", WBYTES)

        for i_ in range(2):
            P.op("pool", lambda h, i_=i_: h.memset(vm1[i_][:, :, 128:129], 1.0), w=["vm1_%d" % i_])

        def rsqrt_col(dst, src, scale, keys_r, key_w, tmpcol):
            P.op("act", lambda h: h.activation(out=tmpcol, in_=src, func=AF.Ln, scale=float(scale), bias=EPS),
                 r=keys_r, w=[key_w + "_sq"])
            P.op("act", lambda h: h.activation(out=dst, in_=tmpcol, func=AF.Exp, scale=-0.5), r=[key_w + "_sq"], w=[key_w])

        x_v = x_d.rearrange("(t p) n -> t p n", p=128)
        x1_v = x1_d.rearrange("(t p) n -> t p n", p=128)
        out_v = out_d.rearrange("(t p) n -> t p n", p=128)

        FL = {}

        def load_x(t):
            s = t % 3
            P.dma("sp", lambda h: h.dma_start(out=X[s], in_=x_v[t]), "X%d" % s, w=["X%d" % s])

        def make_tile(t):
            b = t // TPS
            n = t % TPS
            s = t % 3
            p = t % 2
            p3 = t % 3
            q3 = (t - 1) % 3
            Xk = "X%d" % s
            hT_, K_hT = hT[p], "hT%d" % p
            qaT_, K_qaT = qaT[p], "qaT%d" % p
            km_tm_, K_kmtm = km_tm[p], "km_tm%d" % p
            kmT_, K_kmT = kmT[p], "kmT%d" % p
            qmT_, K_qmT = qmT[p], "qmT%d" % p
            vm1_, K_vm1 = vm1[p], "vm1_%d" % p
            sigO_, gs_, K_sigO = sigO[p], gs[p], "sigO%d" % p
            gi_, K_gi = gi[p], "gi%d" % p

            def wait_flag(key):
                while not FL.get(key):
                    yield

            def proj(c0, n_, ps_ap, keys):
                for k in range(8):
                    P.op("pe", lambda h, k=k: h.matmul(ps_ap[:, 0:n_], lhsT=hT_[:, k, :], rhs=win_sb[:, k, c0:c0 + n_],
                                                       start=(k == 0), stop=(k == 7)),
                         r=[K_hT, "win%d" % k], w=keys, track_reads=True)

            def g_front():

                P.op("act", lambda h: h.activation(out=xs, in_=X[s], func=AF.Square, accum_out=st8[:, 0:1]),
                     r=[Xk], w=["xs", "ssq1"])
                yield
                rsqrt_col(st8[:, 2:3], st8[:, 0:1], 1.0 / D, ["ssq1"], "rstd1", st8[:, 1:2])
                yield
                P.op("dve", lambda h: h.tensor_scalar(out=xs, in0=X[s], scalar1=st8[:, 2:3], scalar2=None, op0=ALU.mult),
                     r=[Xk, "rstd1"], w=["xs"])
                yield
                ps, pk = yield from gbank()
                psb = ps.bitcast(BF16).rearrange("p (k n) -> p k n", k=8)
                for k in range(8):
                    P.op("pe", lambda h, k=k: h.transpose(out=psb[:, k, :], in_=xs[:, k * 128:(k + 1) * 128], identity=ident_b),
                         r=["xs", "ident_b"], w=pk)
                    yield
                for k in range(8):
                    P.op("act", lambda h, k=k: h.activation(out=hT_[:, k, :], in_=psb[:, k, :], func=AF.Identity,
                                                            bias=S1c[:, k, b:b + 1], scale=A1c[:, k, b:b + 1]),
                         r=pk + ["adacol0", "adacol1"], w=[K_hT])
                    yield
                rel(pk)
                ps_c, k_c = yield from gbank()
                proj(1024, 256, ps_c, k_c)
                yield
                proj(2304, 8, ps_c[:, 256:264], k_c)
                yield
                P.op("dve", lambda h: h.tensor_tensor(out=gi_, in0=ps_c[:, 256:264], in1=vr[:, V_BG:V_BG + 8], op=ALU.add),
                     r=k_c + ["vr"], w=[K_gi])
                yield
                FL[(t, "gi")] = True
                P.op("act", lambda h: h.copy(out=km_tm_, in_=ps_c[:, 0:256].rearrange("p (h d) -> p h d", h=4)), r=k_c, w=[K_kmtm])
                yield
                rel(k_c)
                ps_qa, k_qa = yield from gbank()
                proj(0, 512, ps_qa, k_qa)
                yield
                qa3 = ps_qa.rearrange("p (h d) -> p h d", h=8)
                P.op("act", lambda h: h.copy(out=qkf[:, 0:8, :], in_=qa3), r=k_qa, w=["qkf"])
                yield
                rel(k_qa)
                ps_b, k_b = yield from gbank()
                proj(512, 512, ps_b, k_b)
                yield
                ka3 = ps_b[:, 0:128].rearrange("p (h d) -> p h d", h=2)
                P.op("act", lambda h: h.copy(out=qkf[:, 8:10, :], in_=ka3), r=k_b, w=["qkf"])
                yield
                va3 = ps_b[:, 128:256].rearrange("p (h d) -> p h d", h=2)
                for dup in range(2):
                    P.op("dve", lambda h, dup=dup: h.tensor_copy(out=vaD[p3][:, :, dup, :], in_=va3), r=k_b, w=["vaD%d" % p3])
                    yield
                P.op("act", lambda h: h.copy(out=qm_tm, in_=ps_b[:, 256:512].rearrange("p (h d) -> p h d", h=4)), r=k_b, w=["qm_tm"])
                yield
                rel(k_b)
                P.op("dve", lambda h: h.tensor_copy(out=qk_tm, in_=qkf), r=["qkf"], w=["qk_tm"])
                yield
                ps_vm, k_vm = yield from gbank()
                proj(1280, 512, ps_vm, k_vm)
                yield
                P.op("dve", lambda h: h.tensor_copy(out=vm1_[:, :, 0:128], in_=ps_vm.rearrange("p (h d) -> p h d", h=4)),
                     r=k_vm, w=[K_vm1])
                yield
                rel(k_vm)
                ps_om, k_om = yield from gbank()
                proj(1792, 512, ps_om, k_om)
                yield
                P.op("act", lambda h: h.activation(out=sigO_, in_=ps_om, func=AF.Exp, scale=-1.0), r=k_om, w=[K_sigO])
                yield
                rel(k_om)
                P.op("act", lambda h: h.activation(out=sigO_, in_=sigO_, func=AF.Ln, bias=1.0), r=[K_sigO], w=[K_sigO])
                yield
                P.op("act", lambda h: h.activation(out=sigO_, in_=sigO_, func=AF.Exp, scale=-1.0), r=[K_sigO], w=[K_sigO])
                yield
                P.op("pool", lambda h: h.tensor_tensor(out=gs_, in0=sigO_.rearrange("p (h d) -> p h d", h=4),
                                                       in1=vr[:, V_GN:V_GN + 512].rearrange("p (h d) -> p h d", h=4), op=ALU.mult),
                     r=[K_sigO, "vr"], w=[K_sigO])
                yield
                cs = cosT[:, t, :].unsqueeze(1)
                sn = sinT[:, t, :].unsqueeze(1)
                for (src, h0, nh, kk) in ((qkf[:, 0:8, :], 0, 8, ["qkf"]), (qkf[:, 8:10, :], 8, 2, ["qkf"])):
                    cb = cs.to_broadcast([128, nh, 8])
                    sb = sn.to_broadcast([128, nh, 8])
                    x1_ = src[:, :, 0:8]
                    x2_ = src[:, :, 8:16]
                    r0 = rt[:, 0, h0:h0 + nh, :]
                    r1 = rt[:, 1, h0:h0 + nh, :]
                    r2 = rt[:, 2, h0:h0 + nh, :]
                    r3 = rt[:, 3, h0:h0 + nh, :]
                    tg = "rt%d" % h0
                    P.op("dve", lambda h, r0=r0, x1_=x1_, cb=cb: h.tensor_tensor(out=r0, in0=x1_, in1=cb, op=ALU.mult),
                         r=kk + ["cosT"], w=[tg + "a"])
                    yield
                    P.op("dve", lambda h, r1=r1, x2_=x2_, sb=sb: h.tensor_tensor(out=r1, in0=x2_, in1=sb, op=ALU.mult),
                         r=kk + ["sinT"], w=[tg + "b"])
                    yield
                    P.op("dve", lambda h, r2=r2, x2_=x2_, cb=cb: h.tensor_tensor(out=r2, in0=x2_, in1=cb, op=ALU.mult),
                         r=kk + ["cosT"], w=[tg + "c"])
                    yield
                    P.op("dve", lambda h, r3=r3, x1_=x1_, sb=sb: h.tensor_tensor(out=r3, in0=x1_, in1=sb, op=ALU.mult),
                         r=kk + ["sinT"], w=[tg + "d"])
                    yield
                    P.op("dve", lambda h, r0=r0, r1=r1, h0=h0, nh=nh: h.tensor_tensor(out=qk_tm[:, h0:h0 + nh, 0:8], in0=r0, in1=r1,
                                                                                    op=ALU.subtract),
                         r=[tg + "a", tg + "b"], w=["qk_tm"])
                    yield
                    P.op("dve", lambda h, r2=r2, r3=r3, h0=h0, nh=nh: h.tensor_tensor(out=qk_tm[:, h0:h0 + nh, 8:16], in0=r2, in1=r3,
                                                                                    op=ALU.add),
                         r=[tg + "c", tg + "d"], w=["qk_tm"])
                    yield
                ps1, k1 = yield from gbank()
                p1b = ps1.bitcast(BF16)
                qaT_ps = p1b[0:64, :].rearrange("p (h n) -> p h n", h=8)
                for hh in range(8):
                    P.op("pe", lambda h, hh=hh: h.transpose(out=qaT_ps[:, hh, :], in_=qk_tm[:, hh, :], identity=ident_b),
                         r=["qk_tm", "ident_b"], w=k1)
                    yield
                P.op("act", lambda h: h.copy(out=qaT_, in_=qaT_ps), r=k1, w=[K_qaT])
                yield
                rel(k1)
                ps2, k2 = yield from gbank()
                p2b = ps2.bitcast(BF16)
                kaT_ps = p2b[0:64, 0:256].rearrange("p (h n) -> p h n", h=2)
                kmT_ps = p2b[0:64, 256:768].rearrange("p (h n) -> p h n", h=4)
                for hh in range(2):
                    P.op("pe", lambda h, hh=hh: h.transpose(out=kaT_ps[:, hh, :], in_=qk_tm[:, 8 + hh, :], identity=ident_b),
                         r=["qk_tm", "ident_b"], w=k2)
                    yield
                for hh in range(4):
                    P.op("pe", lambda h, hh=hh: h.transpose(out=kmT_ps[:, hh, :], in_=km_tm_[:, hh, :], identity=ident_b),
                         r=[K_kmtm, "ident_b"], w=k2)
                    yield
                P.op("dve", lambda h: h.tensor_copy(out=kaT[p3], in_=kaT_ps), r=k2, w=["kaT%d" % p3])
                yield
                P.op("dve", lambda h: h.tensor_copy(out=kmT_, in_=kmT_ps), r=k2, w=[K_kmT])
                yield
                rel(k2)
                ps3, k3 = yield from gbank()
                p3b = ps3.bitcast(BF16)
                qmT_ps = p3b[0:64, 0:512].rearrange("p (h n) -> p h n", h=4)
                for hh in range(4):
                    P.op("pe", lambda h, hh=hh: h.transpose(out=qmT_ps[:, hh, :], in_=qm_tm[:, hh, :], identity=ident_b),
                         r=["qm_tm", "ident_b"], w=k3)
                    yield
                P.op("act", lambda h: h.copy(out=qmT_, in_=qmT_ps), r=k3, w=[K_qmT])
                yield
                rel(k3)


            def g_gates():

                for c in range(4):
                    if t > 0 and c == 0:
                        yield from wait_flag((t - 1, "t12_done"))
                    psg, kg = yield from gbank()
                    proj(2312 + c * 512, 512, psg, kg)
                    yield
                    sgc = sigG[:, c * 512:(c + 1) * 512]
                    P.op("act", lambda h, sgc=sgc, psg=psg: h.activation(out=sgc, in_=psg, func=AF.Exp, scale=-1.0),
                         r=kg, w=["sigG%d" % c])
                    yield
                    rel(kg)
                    P.op("act", lambda h, sgc=sgc: h.activation(out=sgc, in_=sgc, func=AF.Ln, bias=1.0), r=["sigG%d" % c], w=["sigG%d" % c])
                    yield
                    P.op("act", lambda h, sgc=sgc: h.activation(out=sgc, in_=sgc, func=AF.Exp, scale=-1.0), r=["sigG%d" % c], w=["sigG%d" % c])
                    yield


            def g_att(hk):

                blocks = [(p3, mcur_b, "mcur_b")]
                if n > 0:
                    blocks.append((q3, mprev_b, "mprev_b"))
                ps_o, k_o = yield from gbank()
                ps_d, k_d = yield from gbank()
                for bi, (slot, mask, mkey) in enumerate(blocks):
                    ps_s, k_s = yield from gbank()
                    P.op("pe", lambda h, slot=slot, hk=hk, ps_s=ps_s: h.matmul(
                        ps_s, lhsT=kaT[slot][:, hk, :], rhs=qaT_[:, 4 * hk:4 * hk + 4, :].rearrange("p a b -> p (a b)"), start=True, stop=True),
                        r=["kaT%d" % slot, K_qaT], w=k_s)
                    yield
                    P.op("act", lambda h, bi=bi, ps_s=ps_s: h.activation(out=eT[bi], in_=ps_s.rearrange("p (g n) -> p g n", g=4),
                                                                         func=AF.Exp, scale=0.125),
                         r=k_s, w=["eT%d" % bi])
                    yield
                    rel(k_s)
                    P.op("dve", lambda h, bi=bi, mask=mask: h.tensor_tensor(out=pT[bi], in0=eT[bi],
                                                                             in1=mask.unsqueeze(1).to_broadcast([128, 4, 128]),
                                                                             op=ALU.mult),
                         r=["eT%d" % bi, mkey], w=["pT%d" % bi])
                    yield
                    P.op("pe", lambda h, slot=slot, hk=hk, bi=bi, ps_o=ps_o: h.matmul(
                        ps_o, lhsT=vaD[slot][:, hk, :, :].rearrange("p a b -> p (a b)"), rhs=pT[bi].rearrange("p a b -> p (a b)"), start=(bi == 0), stop=(bi == len(blocks) - 1)),
                        r=["vaD%d" % slot, "pT%d" % bi], w=k_o)
                    yield
                    P.op("pe", lambda h, bi=bi, ps_d=ps_d: h.matmul(
                        ps_d, lhsT=ones_b, rhs=pT[bi].rearrange("p a b -> p (a b)"), start=(bi == 0), stop=(bi == len(blocks) - 1)),
                        r=["ones_b", "pT%d" % bi], w=k_d)
                    yield
                es4 = esink[:, 4 * hk:4 * hk + 4].unsqueeze(2).to_broadcast([128, 4, 128])
                P.op("dve", lambda h, ps_d=ps_d, es4=es4: h.tensor_tensor(out=den2, in0=ps_d.rearrange("p (g n) -> p g n", g=4),
                                                                         in1=es4, op=ALU.add),
                     r=k_d + ["esink"], w=["den2"])
                yield
                rel(k_d)
                P.op("act", lambda h: h.activation(out=rec, in_=den2, func=AF.Ln), r=["den2"], w=["den2"])
                yield
                P.op("act", lambda h: h.activation(out=rec, in_=rec, func=AF.Exp, scale=-1.0), r=["den2"], w=["den2"])
                yield
                if t > 0:
                    yield from wait_flag((t - 1, "a_done"))
                o4 = ps_o.rearrange("p (j g n) -> p j g n", j=2, g=2)
                r4 = rec.rearrange("p (j g) n -> p j g n", j=2)
                for half in range(2):
                    lo, hi = half * 64, half * 64 + 64
                    for j in range(2):
                        P.op("dve", lambda h, lo=lo, hi=hi, half=half, j=j, o4=o4, r4=r4, hk=hk: h.tensor_tensor(
                            out=attT[lo:hi, 2 * hk + j, :], in0=o4[lo:hi, j, half, :], in1=r4[lo:hi, j, half, :], op=ALU.mult),
                            r=k_o + ["den2"], w=["attT"])
                        yield
                rel(k_o)


            def g_mlg():
                yield from wait_flag((t, "gi"))
                if t > 0:
                    yield from wait_flag((t - 1, "dexp_done"))

                P.op("act", lambda h: h.activation(out=th, in_=gi_, func=AF.Exp, scale=2.0 / 15), r=[K_gi], w=["th"])
                yield
                P.op("dve", lambda h: h.tensor_scalar(out=th, in0=th, scalar1=1.0, scalar2=None, op0=ALU.add), r=["th"], w=["th"])
                yield
                P.op("dve", lambda h: h.reciprocal(out=th, in_=th), r=["th"], w=["th"])
                yield
                P.op("dve", lambda h: h.tensor_scalar(out=th, in0=th, scalar1=-2.0, scalar2=1.0, op0=ALU.mult, op1=ALU.add), r=["th"], w=["th"])
                yield
                P.op("act", lambda h: h.activation(out=ef, in_=th[:, 4:8], func=AF.Exp, scale=-15.0), r=["th"], w=["ef"])
                yield
                P.op("act", lambda h: h.activation(out=ef, in_=ef, func=AF.Ln, bias=1.0), r=["ef"], w=["ef"])
                yield
                P.op("dve", lambda h: h.tensor_scalar(out=Fl, in0=ef, scalar1=-1.0, scalar2=None, op0=ALU.mult), r=["ef"], w=["Fl"])
                yield
                ps_cs, k_cs = yield from gbank()
                P.op("pe", lambda h: h.matmul(ps_cs[:, 0:4], lhsT=tri_f, rhs=Fl, start=True, stop=True), r=["cf", "Fl"], w=k_cs)
                yield
                P.op("dve", lambda h: h.scalar_tensor_tensor(out=imb, in0=th[:, 0:4], scalar=15.0, in1=ps_cs[:, 0:4],
                                                             op0=ALU.mult, op1=ALU.subtract), r=["th"] + k_cs, w=["imb"])
                yield
                rel(k_cs)
                P.op("dve", lambda h: h.tensor_tensor(out=Fm, in0=tri_f.unsqueeze(1).to_broadcast([128, 4, 128]),
                                                      in1=Fl.unsqueeze(2).to_broadcast([128, 4, 128]), op=ALU.mult),
                     r=["cf", "Fl"], w=["Fm"])
                yield
                ps_rb, k_rb = yield from gbank()
                P.op("pe", lambda h: h.matmul(ps_rb, lhsT=ones_f, rhs=Fm.rearrange("p a b -> p (a b)"), start=True, stop=True), r=["cf", "Fm"], w=k_rb)
                yield
                rb3 = ps_rb.rearrange("p (g n) -> p g n", g=4)
                for hh in range(4):
                    P.op("act", lambda h, hh=hh: h.activation(out=Dexp[:, hh, :], in_=rb3[:, hh, :], func=AF.Exp, bias=imb[:, hh:hh + 1]),
                         r=k_rb + ["imb"], w=["Dexp"])
                    yield
                P.op("act", lambda h: h.activation(out=Eb, in_=rb3[0:64], func=AF.Exp), r=k_rb, w=["Eb"])
                yield
                rel(k_rb)

            def g_ml():
                P.op("dve", lambda h: h.scalar_tensor_tensor(out=qsT, in0=qmT_, scalar=0.125, in1=Eb, op0=ALU.mult, op1=ALU.mult),
                     r=[K_qmT, "Eb"], w=["qsT"])
                yield
                ps_st, k_st = yield from gbank()
                st3 = ps_st.rearrange("p (g n) -> p g n", g=4)
                for hh in range(4):
                    P.op("pe", lambda h, hh=hh: h.matmul(st3[:, hh, :], lhsT=kmT_[:, hh, :], rhs=qmT_[:, hh, :], start=True, stop=True),
                         r=[K_kmT, K_qmT], w=k_st)
                    yield
                P.op("dve", lambda h: h.tensor_tensor(out=tmpW, in0=Dexp, in1=maskw_f.unsqueeze(1).to_broadcast([128, 4, 128]),
                                                       op=ALU.mult), r=["Dexp", "cf"], w=["Fm"])
                yield
                P.op("dve", lambda h: h.tensor_tensor(out=WT, in0=tmpW, in1=st3, op=ALU.mult), r=["Fm"] + k_st, w=["WT"])
                yield
                rel(k_st)
                P.op("pool", lambda h: h.tensor_tensor(out=kw, in0=km_tm_, in1=Dexp[:, :, 127:128].to_broadcast([128, 4, 64]),
                                                       op=ALU.mult), r=[K_kmtm, "Dexp"], w=["kw"])
                yield
                nums = []
                for pr in range(2):
                    ps_n, k_n = yield from gbank()
                    n3 = ps_n.rearrange("p (g n) -> p g n", g=2)[:, :, 0:129]
                    for g in range(2):
                        hh = 2 * pr + g
                        P.op("pe", lambda h, hh=hh, g=g, n3=n3: h.matmul(n3[:, g, :], lhsT=WT[:, hh, :], rhs=vm1_[:, hh, :],
                                                                          start=True, stop=(n == 0)),
                             r=["WT", K_vm1], w=k_n)
                        yield
                        if n > 0:
                            P.op("pe", lambda h, hh=hh, g=g, n3=n3: h.matmul(n3[:, g, :], lhsT=qsT[:, hh, :], rhs=C1b[:, hh, :],
                                                                              start=False, stop=True),
                                 r=["qsT", "C1b"], w=k_n)
                            yield
                    nums.append((n3, k_n))
                cps = []
                for pr in range(2):
                    ps_c2, k_c2 = yield from gbank()
                    c3 = ps_c2[0:64, :].rearrange("p (g n) -> p g n", g=2)[:, :, 0:129]
                    for g in range(2):
                        hh = 2 * pr + g
                        P.op("pe", lambda h, hh=hh, g=g, c3=c3: h.matmul(c3[:, g, :], lhsT=kw[:, hh, :], rhs=vm1_[:, hh, :],
                                                                          start=True, stop=True),
                             r=["kw", K_vm1], w=k_c2)
                        yield
                    cps.append((c3, k_c2))
                if n > 0:
                    P.op("dve", lambda h: h.tensor_tensor(out=C1, in0=C1, in1=Eb[:, :, 127:128].to_broadcast([64, 4, 129]), op=ALU.mult),
                         r=["C1", "Eb"], w=["C1"])
                    yield
                    for pr in range(2):
                        c3, k_c2 = cps[pr]
                        for g in range(2):
                            P.op("dve", lambda h, pr=pr, g=g, c3=c3: h.tensor_tensor(out=C1[:, 2 * pr + g, :], in0=C1[:, 2 * pr + g, :],
                                                                                     in1=c3[:, g, :], op=ALU.add), r=["C1"] + k_c2, w=["C1"])
                            yield
                else:
                    for pr in range(2):
                        c3, k_c2 = cps[pr]
                        for g in range(2):
                            P.op("dve", lambda h, pr=pr, g=g, c3=c3: h.tensor_copy(out=C1[:, 2 * pr + g, :], in_=c3[:, g, :]), r=k_c2, w=["C1"])
                            yield
                FL[(t, "dexp_done")] = True
                P.op("pool", lambda h: h.tensor_copy(out=C1b, in_=C1), r=["C1"], w=["C1b"])
                yield
                for (_c3, _k) in cps:
                    rel(_k)
                for pr in range(2):
                    n3, k_n = nums[pr]
                    for g in range(2):
                        P.op("dve", lambda h, pr=pr, g=g, n3=n3: h.tensor_copy(out=dd[:, 2 * pr + g:2 * pr + g + 1], in_=n3[:, g, 128:129]),
                             r=k_n, w=["dd"])
                        yield
                    for g in range(2):
                        hh = 2 * pr + g
                        P.op("act", lambda h, hh=hh, g=g, n3=n3: h.activation(out=junk, in_=n3[:, g, 0:128], func=AF.Square,
                                                                              accum_out=ssq4[:, hh:hh + 1]),
                             r=k_n, w=["junk", "ssq4"])
                        yield
                sA = sm4[:, 0, :]
                sB = sm4[:, 1, :]
                sC = sm4[:, 2, :]
                sS = sm4[:, 3, :]
                P.op("dve", lambda h: h.scalar_tensor_tensor(out=dd, in0=dd, scalar=-1.0, in1=dd, op0=ALU.mult, op1=ALU.max), r=["dd"], w=["dd"])
                yield
                P.op("dve", lambda h: h.tensor_scalar(out=dd, in0=dd, scalar1=1.0, scalar2=None, op0=ALU.max), r=["dd"], w=["dd"])
                yield
                P.op("dve", lambda h: h.reciprocal(out=sA, in_=dd), r=["dd"], w=["sA"])
                yield
                P.op("dve", lambda h: h.tensor_tensor(out=sB, in0=sA, in1=sA, op=ALU.mult), r=["sA"], w=["sB"])
                yield
                P.op("dve", lambda h: h.tensor_tensor(out=sB, in0=sB, in1=ssq4, op=ALU.mult), r=["sB", "ssq4"], w=["sB"])
                yield
                P.op("act", lambda h: h.activation(out=sC, in_=sB, func=AF.Ln, scale=1.0 / 128, bias=EPS), r=["sB"], w=["sC"])
                yield
                P.op("act", lambda h: h.activation(out=sC, in_=sC, func=AF.Exp, scale=-0.5), r=["sC"], w=["sC"])
                yield
                P.op("dve", lambda h: h.tensor_tensor(out=sS, in0=sA, in1=sC, op=ALU.mult), r=["sA", "sC"], w=["sS"])
                yield
                for pr in range(2):
                    n3, k_n = nums[pr]
                    for g in range(2):
                        hh = 2 * pr + g
                        P.op("dve", lambda h, hh=hh, g=g, n3=n3: h.scalar_tensor_tensor(out=ml[:, hh, :], in0=n3[:, g, 0:128],
                                                                                        scalar=sm4[:, 3, hh:hh + 1], in1=gs_[:, hh, :],
                                                                                        op0=ALU.mult, op1=ALU.mult),
                             r=k_n + ["sS", K_sigO], w=["ml"])
                        yield
                for (_n3, _k) in nums:
                    rel(_k)
                ps_mt, k_mt = yield from gbank()
                mlT_ps = ps_mt.bitcast(BF16)[:, 0:512].rearrange("p (g n) -> p g n", g=4)
                for hh in range(4):
                    P.op("pe", lambda h, hh=hh: h.transpose(out=mlT_ps[:, hh, :], in_=ml[:, hh, :], identity=ident_b),
                         r=["ml", "ident_b"], w=k_mt)
                    yield
                if t > 0:
                    yield from wait_flag((t - 1, "mo_done"))
                P.op("act", lambda h: h.copy(out=mlT, in_=mlT_ps), r=k_mt, w=["mlT"])
                yield
                rel(k_mt)


            def g_tail():

                ps_a, k_a = yield from gbank(2)
                for c in range(2):
                    for j in range(4):
                        P.op("pe", lambda h, c=c, j=j: h.matmul(ps_a[:, c * 512:(c + 1) * 512], lhsT=attT[:, j, :],
                                                                rhs=wao_sb[:, j, c * 512:(c + 1) * 512], start=(j == 0), stop=(j == 3)),
                             r=["attT", "wao"], w=[k_a[c]])
                        yield
                FL[(t, "a_done")] = True
                P.op("dve", lambda h: h.tensor_tensor(out=t1, in0=ps_a, in1=sigG[:, 0:1024], op=ALU.mult),
                     r=k_a + ["sigG0", "sigG1"], w=["t1"])
                yield
                rel(k_a)
                ps_m, k_m = yield from gbank(2)
                for c in range(2):
                    for j in range(4):
                        P.op("pe", lambda h, c=c, j=j: h.matmul(ps_m[:, c * 512:(c + 1) * 512], lhsT=mlT[:, j, :],
                                                                rhs=wmo_sb[:, j, c * 512:(c + 1) * 512], start=(j == 0), stop=(j == 3)),
                             r=["mlT", "wmo"], w=[k_m[c]])
                        yield
                FL[(t, "mo_done")] = True
                P.op("dve", lambda h: h.tensor_tensor(out=t2, in0=ps_m, in1=sigG[:, 1024:2048], op=ALU.mult),
                     r=k_m + ["sigG2", "sigG3"], w=["x1t"])
                yield
                rel(k_m)
                FL[(t, "t12_done")] = True
                P.op("dve", lambda h: h.tensor_tensor(out=merged, in0=t1, in1=t2, op=ALU.add), r=["t1", "x1t"], w=["merged"])
                yield
                ps_t, k_t = yield from gbank()
                mT_ps = ps_t.bitcast(BF16).rearrange("p (k n) -> p k n", k=8)
                for k in range(8):
                    P.op("pe", lambda h, k=k: h.transpose(out=mT_ps[:, k, :], in_=merged[:, k * 128:(k + 1) * 128], identity=ident_b),
                         r=["merged", "ident_b"], w=k_t)
                    yield
                P.op("act", lambda h: h.copy(out=mT, in_=mT_ps), r=k_t, w=["mT"])
                yield
                rel(k_t)
                ps_x, k_x = yield from gbank(2)
                for c in range(2):
                    for k in range(8):
                        P.op("pe", lambda h, c=c, k=k: h.matmul(ps_x[:, c * 512:(c + 1) * 512], lhsT=mT[:, k, :],
                                                                rhs=wout_sb[:, k, c * 512:(c + 1) * 512], start=(k == 0), stop=(k == 7)),
                             r=["mT", "wout"], w=[k_x[c]])
                        yield
                P.op("act", lambda h: h.activation(out=merged, in_=ps_x, func=AF.Square, accum_out=st8[:, 4:5]), r=k_x, w=["merged", "ssq2"])
                yield
                rsqrt_col(st8[:, 6:7], st8[:, 4:5], 1.0 / D, ["ssq2"], "rstd2", st8[:, 5:6])
                yield
                P.op("dve", lambda h: h.scalar_tensor_tensor(out=t1, in0=ps_x, scalar=st8[:, 6:7], in1=G1r[:, b, :],
                                                             op0=ALU.mult, op1=ALU.mult),
                     r=k_x + ["rstd2", "adarow2_%d" % b], w=["t1"])
                yield
                rel(k_x)
                P.op("dve", lambda h: h.tensor_tensor(out=x1t, in0=X[s], in1=t1, op=ALU.add), r=[Xk, "t1"], w=["x1t"])
                yield
                P.dma("sp", lambda h: h.dma_start(out=x1_v[t], in_=x1t), "x1t", r=["x1t"], w=["x1d%d" % t],
                      final=(not do_ffn))
                yield


            return g_front, g_gates, g_att, g_ml, g_tail, g_mlg

        def interleave(gens):
            gens = list(gens)
            while gens:
                for g in list(gens):
                    try:
                        next(g)
                    except StopIteration:
                        gens.remove(g)

        tiles = [make_tile(t) for t in range(n_tiles)]

        def g_att_all(g_att):
            yield from g_att(0)
            yield from g_att(1)

        load_x(0)
        if n_tiles > 1:
            load_x(1)
        interleave([tiles[0][0](), tiles[0][5]()])
        for t in range(n_tiles):
            g_front, g_gates, g_att, g_ml, g_tail, g_mlg = tiles[t]
            th_ = []
            if t > 0:
                th_.append(tiles[t - 1][4]())
            th_ += [g_ml(), g_att_all(g_att), g_gates()]
            if t + 1 < n_tiles:
                th_ += [tiles[t + 1][0](), tiles[t + 1][5]()]
            interleave(th_)
            if t + 2 < n_tiles:
                load_x(t + 2)
        interleave([tiles[n_tiles - 1][4]()])

        if do_ffn:
            P.barrier()
            for i_ in range(8):
                bank_free[i_] = True
            off[0] = persistB_end
            woff[0] = 0
            wfi_sb = carve([128, 8, 2 * DFF], BF16, warena, woff)
            wfo_sb = carve([128, NF, D], BF16, warena, woff)
            wfi_v = wfi_d.rearrange("(k p) n -> p k n", p=128)
            wfo_v = wfo_d.rearrange("(k p) n -> p k n", p=128)
            FB = 6
            blocks_f = [(f0, min(NF, f0 + FB)) for f0 in range(0, NF, FB)]
            for bi_, (f0, f1) in enumerate(blocks_f):
                for nm, cofs in (("wfg", 0), ("wfu", DFF)):
                    P.dma("pool", lambda h, f0=f0, f1=f1, cofs=cofs: h.dma_start(
                        out=wfi_sb[:, :, cofs + f0 * 128:cofs + f1 * 128], in_=wfi_v[:, :, cofs + f0 * 128:cofs + f1 * 128]),
                        "%s%d" % (nm, bi_), w=["%s%d" % (nm, bi_)])
            for bi_, (f0, f1) in enumerate(blocks_f):
                P.dma("pool", lambda h, f0=f0, f1=f1: h.dma_start(out=wfo_sb[:, f0:f1, :], in_=wfo_v[:, f0:f1, :]),
                      "wfo%d" % bi_, w=["wfo%d" % bi_])

            GT = 4
            XB = [carve([128, D], F32) for _ in range(2)]
            sb8 = carve([128, 16], F32)
            xs2 = carve([128, D], BF16)
            h2T = [carve([128, 8, GT * 128], BF16) for _ in range(2)]
            actT = carve([128, NF, GT * 128], BF16)
            silu = [carve([128, GT * 128], F32), carve([128, GT * 128], F32)]
            junk2 = silu[0].bitcast(BF16)
            ot = [carve([128, D], F32), carve([128, D], F32)]
            rx = carve([128, D], F32)

            def g_prep(g_):
                b = (g_ * GT) // TPS
                hb = g_ % 2
                for j in range(GT):
                    t = g_ * GT + j
                    xb = XB[j % 2]
                    xk = "XB%d" % (j % 2)
                    P.dma("sp", lambda h, t=t, xb=xb: h.dma_start(out=xb, in_=x1_v[t]), xk, r=["x1d%d" % t], w=[xk])
                    yield
                    P.op("act", lambda h, xb=xb: h.activation(out=xs2, in_=xb, func=AF.Square, accum_out=sb8[:, 0:1]),
                         r=[xk], w=["xs2", "b_ssq"])
                    yield
                    rsqrt_col(sb8[:, 2:3], sb8[:, 0:1], 1.0 / D, ["b_ssq"], "b_rstd", sb8[:, 1:2])
                    yield
                    P.op("dve", lambda h, xb=xb: h.tensor_scalar(out=xs2, in0=xb, scalar1=sb8[:, 2:3], scalar2=None, op0=ALU.mult),
                         r=[xk, "b_rstd"], w=["xs2"])
                    yield
                    ps, pk = yield from gbank()
                    psb = ps.bitcast(BF16).rearrange("p (k n) -> p k n", k=8)
                    for k in range(8):
                        P.op("pe", lambda h, k=k, psb=psb: h.transpose(out=psb[:, k, :], in_=xs2[:, k * 128:(k + 1) * 128], identity=ident_b),
                             r=["xs2", "ident_b"], w=pk)
                        yield
                    for k in range(8):
                        P.op("act", lambda h, k=k, j=j, psb=psb, b=b, hb=hb: h.activation(
                            out=h2T[hb][:, k, j * 128:(j + 1) * 128], in_=psb[:, k, :], func=AF.Identity,
                            bias=S2c[:, k, b:b + 1], scale=A2c[:, k, b:b + 1]),
                            r=pk + ["adacol3", "adacol4"], w=["h2T%d" % hb])
                        yield
                    rel(pk)

            def g_ffn(g_):
                b = (g_ * GT) // TPS
                hb = g_ % 2
                hk_ = "h2T%d" % hb
                for f in range(NF):
                    blk = f // FB
                    ps_g, k_g = yield from gbank()
                    for k in range(8):
                        P.op("pe", lambda h, k=k, f=f, ps_g=ps_g: h.matmul(ps_g, lhsT=wfi_sb[:, k, f * 128:(f + 1) * 128],
                                                                           rhs=h2T[hb][:, k, :], start=(k == 0), stop=(k == 7)),
                             r=["wfg%d" % blk, hk_], w=k_g)
                    yield
                    ps_u, k_u = yield from gbank()
                    for k in range(8):
                        P.op("pe", lambda h, k=k, f=f, ps_u=ps_u: h.matmul(ps_u, lhsT=wfi_sb[:, k, DFF + f * 128:DFF + (f + 1) * 128],
                                                                           rhs=h2T[hb][:, k, :], start=(k == 0), stop=(k == 7)),
                             r=["wfu%d" % blk, hk_], w=k_u)
                    yield
                    sl = silu[f % 2]
                    P.op("act", lambda h, sl=sl, ps_g=ps_g: h.activation(out=sl, in_=ps_g, func=AF.Silu), r=k_g, w=["silu%d" % (f % 2)])
                    yield
                    rel(k_g)
                    P.op("dve", lambda h, sl=sl, f=f, ps_u=ps_u: h.tensor_tensor(out=actT[:, f, :], in0=sl, in1=ps_u, op=ALU.mult),
                         r=["silu%d" % (f % 2)] + k_u, w=["actT%d" % f])
                    yield
                    rel(k_u)
                for j in range(GT):
                    t = g_ * GT + j
                    ps_y, k_y = yield from gbank(2)
                    for c in range(2):
                        for f in range(NF):
                            P.op("pe", lambda h, c=c, f=f, j=j, ps_y=ps_y: h.matmul(
                                ps_y[:, c * 512:(c + 1) * 512], lhsT=actT[:, f, j * 128:(j + 1) * 128],
                                rhs=wfo_sb[:, f, c * 512:(c + 1) * 512], start=(f == 0), stop=(f == NF - 1)),
                                r=["actT%d" % f, "wfo%d" % (f // FB)], w=[k_y[c]])
                            if f % 4 == 3:
                                yield
                    P.dma("sp", lambda h, t=t: h.dma_start(out=rx, in_=x1_v[t]), "rx", r=["x1d%d" % t], w=["rx"])
                    yield
                    P.op("act", lambda h, ps_y=ps_y: h.activation(out=junk2, in_=ps_y, func=AF.Square, accum_out=sb8[:, 4:5]),
                         r=k_y, w=["silu0", "b_ssq2"])
                    yield
                    rsqrt_col(sb8[:, 6:7], sb8[:, 4:5], 1.0 / D, ["b_ssq2"], "b_rstd2", sb8[:, 5:6])
                    yield
                    o_ = ot[j % 2]
                    P.op("dve", lambda h, ps_y=ps_y, o_=o_, b=b: h.scalar_tensor_tensor(out=o_, in0=ps_y, scalar=sb8[:, 6:7], in1=G2r[:, b, :],
                                                                                   op0=ALU.mult, op1=ALU.mult),
                         r=k_y + ["b_rstd2", "adarow5_%d" % b], w=["ot%d" % (j % 2)])
                    yield
                    rel(k_y)
                    P.op("pool", lambda h, o_=o_: h.tensor_tensor(out=o_, in0=rx, in1=o_, op=ALU.add),
                         r=["rx", "ot%d" % (j % 2)], w=["ot%d" % (j % 2)])
                    yield
                    P.dma("sp", lambda h, o_=o_, t=t: h.dma_start(out=out_v[t], in_=o_), "ot%d" % (j % 2), r=["ot%d" % (j % 2)],
                          final=True)
                    yield

            n_groups = n_tiles // GT
            interleave([g_prep(0)])
            for g_ in range(n_groups):
                th_ = [g_ffn(g_)]
                if g_ + 1 < n_groups:
                    th_.append(g_prep(g_ + 1))
                interleave(th_)

        P.emit(nc, es)
    return nc


def _host_layout(inputs):
    f32 = np.float32
    x = np.asarray(inputs["x"], dtype=f32)
    c = np.asarray(inputs["c"], dtype=f32)
    pos = np.asarray(inputs["positions"], dtype=np.int32)
    cf = np.zeros((128, NCF), f32)
    ii = np.arange(128)
    cf[:, C_ID:C_ID + 128] = np.eye(128, dtype=f32)
    cf[:, C_TRI:C_TRI + 128] = (ii[:, None] <= ii[None, :]).astype(f32)
    cf[:, C_ONE:C_ONE + 128] = 1.0
    cf[:, C_MW:C_MW + 128] = 0.125 * (ii[:, None] <= ii[None, :]).astype(f32)
    cf[:, C_MC:C_MC + 128] = (ii[:, None] <= ii[None, :]).astype(f32)
    cf[:, C_MP:C_MP + 128] = (ii[:, None] > ii[None, :]).astype(f32)
    inv_freq = (np.float32(500000.0) ** (-np.arange(0, 16, 2, dtype=f32) / np.float32(16))).astype(f32)
    cf[:, C_INVF:C_INVF + 8] = inv_freq[None, :]

    def rep(v):
        return np.broadcast_to(np.asarray(v, f32).reshape(1, -1), (128, np.asarray(v).size))

    def col(v):
        return np.asarray(v, f32).reshape(-1, 128).T

    b_ada = np.asarray(inputs["b_ada"], f32)[0]
    vr = np.zeros((128, NVR), f32)
    vr[:, V_BG:V_BG + 8] = rep(inputs["b_mlstm_gates"][0])
    vr[:, V_SINK:V_SINK + 8] = rep(inputs["sinks"][0])
    vr[:, V_GN:V_GN + 512] = rep(inputs["g_mlstm_norm"][0])
    vs = np.zeros((128, NVS), f32)
    vs[:, V_GPM:V_GPM + D] = rep(inputs["g_post_mix"][0])
    vs[:, V_GPF:V_GPF + D] = rep(inputs["g_post_ffn"][0])
    vs[:, V_BG1:V_BG1 + D] = rep(b_ada[2048:3072])
    vs[:, V_BG2:V_BG2 + D] = rep(b_ada[5120:6144])
    vc = np.zeros((128, NVC), f32)
    vc[:, VC_GPM:VC_GPM + 8] = col(inputs["g_pre_mix"][0])
    vc[:, VC_GPF:VC_GPF + 8] = col(inputs["g_pre_ffn"][0])
    vc[:, VC_BSH1:VC_BSH1 + 8] = col(b_ada[0:1024])
    vc[:, VC_BSC1:VC_BSC1 + 8] = col(b_ada[1024:2048])
    vc[:, VC_BSH2:VC_BSH2 + 8] = col(b_ada[3072:4096])
    vc[:, VC_BSC2:VC_BSC2 + 8] = col(b_ada[4096:5120])
    shared = dict(
        cf=cf, vr=vr, vc=vc, vs=vs,
        w_ada=np.ascontiguousarray(inputs["w_ada"][0], dtype=f32),
        w_in=np.ascontiguousarray(inputs["w_in"][0], dtype=f32),
        w_att_o=np.ascontiguousarray(inputs["w_att_o"][0], dtype=f32),
        w_ml_o=np.ascontiguousarray(inputs["w_ml_o"][0], dtype=f32),
        w_out=np.ascontiguousarray(inputs["w_out"][0], dtype=f32),
        w_ffn_in=np.ascontiguousarray(inputs["w_ffn_in"][0], dtype=f32),
        w_ffn_out=np.ascontiguousarray(inputs["w_ffn_out"][0], dtype=f32),
    )
    in_maps = []
    for i in range(NCORES):
        xb = x[2 * i:2 * i + 2].reshape(2 * SEQ, D)
        pb = pos[2 * i:2 * i + 2].reshape(NT, 128).T
        cb = c[2 * i:2 * i + 2]
        ct = cb.reshape(2, 8, 128).transpose(2, 1, 0)
        m = dict(shared)
        m["x"] = np.ascontiguousarray(xb)
        m["pos"] = np.ascontiguousarray(pb, dtype=np.int32)
        m["ct"] = np.ascontiguousarray(ct, dtype=f32)
        in_maps.append(m)
    return in_maps


_NC_CACHE = {}


def kernel(**inputs):
    in_maps = _host_layout(inputs)
    if "nc" not in _NC_CACHE:
        _NC_CACHE["nc"] = build_nc()
    nc = _NC_CACHE["nc"]
    res = run_bass_kernel_spmd(nc, in_maps, core_ids=list(range(NCORES)))
    outs = [np.asarray(r["out"], dtype=np.float32).reshape(2, SEQ, D) for r in res.results]
    return np.concatenate(outs, axis=0)
```
